# Optimizing a Trainium2 kernel written in Bass

```python
import math
import jax
import jax.numpy as jnp
from jax import lax
import numpy as np

D_MODEL = 1024
BATCH = 8
SEQ = 4096
DEPTH = 1

EPS = 1e-6
NEG = -1e30
SGU_WIDTH = 1024
SGU_GROUPS = 8
SGU_GROUP_DIM = SGU_WIDTH // SGU_GROUPS
CHUNK = 128
ATT_HEADS = 8
ATT_HEAD_DIM = 128
ATT_WIDTH = ATT_HEADS * ATT_HEAD_DIM
MOBA_BLOCK = 256
MOBA_TOPK = 3
Q_BLOCK = 64
NUM_BUCKETS = 32
MAX_DISTANCE = 128
IN_WIDTH = 2 * SGU_WIDTH + 3 * ATT_WIDTH
N_BRANCH = 2
MEM_LEN = 256
X_HEADS = 4
X_HEAD_DIM = 128
X_WIDTH = X_HEADS * X_HEAD_DIM
N_GROUPS = 8
EXPERTS_PER_GROUP = 8
N_EXPERTS = N_GROUPS * EXPERTS_PER_GROUP
TOP_K_EXPERTS = 2
EXPERT_HIDDEN = 512
MOE_BLOCK = 256

kernel_name = 'hybrid_sgu_moba_hmoe_block'


def rmsnorm(x, g):
    xf = x.astype(jnp.float32)
    y = xf * lax.rsqrt(jnp.mean(xf * xf, axis=-1, keepdims=True) + EPS)
    return (y * g.astype(jnp.float32)).astype(x.dtype)


def rel_bucket(dist):
    n = jnp.maximum(dist, 0)
    max_exact = NUM_BUCKETS // 2
    nf = jnp.maximum(n, 1).astype(jnp.float32)
    large = max_exact + (jnp.log(nf / max_exact) / math.log(MAX_DISTANCE / max_exact)
                         * (NUM_BUCKETS - max_exact)).astype(jnp.int32)
    large = jnp.minimum(large, NUM_BUCKETS - 1)
    return jnp.where(n < max_exact, n, large)


def chunked_sgu(u_pre, v_pre, sgu_norm, w_spatial, b_spatial):
    B, S, _ = u_pre.shape
    u = jax.nn.gelu(u_pre)
    v = rmsnorm(jax.nn.gelu(v_pre), sgu_norm)
    v = v.reshape(B, S // CHUNK, CHUNK, SGU_GROUPS, SGU_GROUP_DIM)
    causal = jnp.tril(jnp.ones((CHUNK, CHUNK), dtype=bool))
    w = jnp.where(causal[None], w_spatial, jnp.zeros((), w_spatial.dtype))
    mixed = jnp.einsum('gts,bcsgd->bctgd', w, v) + b_spatial.T[None, None, :, :, None]
    return u * mixed.reshape(B, S, SGU_WIDTH)


def moba_attention(q, k, v, rel_bias):
    B, S, H, Dh = q.shape
    Sp = -(-S // MOBA_BLOCK) * MOBA_BLOCK
    pad = ((0, 0), (0, Sp - S), (0, 0), (0, 0))
    q, k, v = jnp.pad(q, pad), jnp.pad(k, pad), jnp.pad(v, pad)
    NB = Sp // MOBA_BLOCK
    NQ = Sp // Q_BLOCK
    K = min(MOBA_TOPK, NB)
    scale = Dh ** -0.5
    kb = k.transpose(0, 2, 1, 3).reshape(B, H, NB, MOBA_BLOCK, Dh)
    vb = v.transpose(0, 2, 1, 3).reshape(B, H, NB, MOBA_BLOCK, Dh)
    kmean = jnp.mean(kb.astype(jnp.float32), axis=3)
    qb = q.transpose(0, 2, 1, 3).reshape(B, H, NQ, Q_BLOCK, Dh).transpose(0, 2, 1, 3, 4)
    bias_hb = rel_bias.T.astype(jnp.float32)
    offs = jnp.arange(MOBA_BLOCK, dtype=jnp.int32)
    h_idx = jnp.arange(H)[:, None, None, None]

    def per_batch(args):
        qb_b, kb_b, vb_b, km_b = args

        def per_qblock(a):
            qblk, iq = a
            t = iq * Q_BLOCK + jnp.arange(Q_BLOCK, dtype=jnp.int32)
            cur = (iq * Q_BLOCK) // MOBA_BLOCK
            gate = jnp.einsum('hqd,hnd->hqn', qblk.astype(jnp.float32), km_b)
            past = jnp.arange(NB) < cur
            gate = jnp.where(past[None, None, :], gate, NEG)
            _, idx = lax.top_k(gate, K)
            valid = jnp.arange(K) < cur
            kg = jax.vmap(lambda kbh, ih: kbh[ih])(kb_b, idx)
            vg = jax.vmap(lambda vbh, ih: vbh[ih])(vb_b, idx)
            s_sel = jnp.einsum('hqd,hqrkd->hqrk', qblk, kg).astype(jnp.float32) * scale
            kpos_sel = idx[..., None] * MOBA_BLOCK + offs
            dist_sel = t[None, :, None, None] - kpos_sel
            s_sel = s_sel + bias_hb[h_idx, rel_bucket(dist_sel)]
            s_sel = jnp.where(valid[None, None, :, None], s_sel, NEG)
            ko = lax.dynamic_index_in_dim(kb_b, cur, axis=1, keepdims=False)
            vo = lax.dynamic_index_in_dim(vb_b, cur, axis=1, keepdims=False)
            s_own = jnp.einsum('hqd,hkd->hqk', qblk, ko).astype(jnp.float32) * scale
            dist_own = t[:, None] - (cur * MOBA_BLOCK + offs)[None, :]
            s_own = s_own + bias_hb[:, rel_bucket(dist_own)]
            s_own = jnp.where(dist_own[None] >= 0, s_own, NEG)
            logits = jnp.concatenate([s_sel.reshape(H, Q_BLOCK, K * MOBA_BLOCK), s_own], axis=-1)
            p = jax.nn.softmax(logits, axis=-1).astype(vb_b.dtype)
            p_sel = p[..., :K * MOBA_BLOCK].reshape(H, Q_BLOCK, K, MOBA_BLOCK)
            p_own = p[..., K * MOBA_BLOCK:]
            return (jnp.einsum('hqrk,hqrkd->hqd', p_sel, vg)
                    + jnp.einsum('hqk,hkd->hqd', p_own, vo))

        return lax.map(per_qblock, (qb_b, jnp.arange(NQ, dtype=jnp.int32)))

    out = lax.map(per_batch, (qb, kb, vb, kmean))
    out = out.transpose(0, 1, 3, 2, 4).reshape(B, Sp, H * Dh)
    return out[:, :S]


def token_mixer(xn, w_in, w_gate, b_gate, sgu_norm, w_spatial, b_spatial,
                w_branch_a, w_branch_b, rel_bias, w_o):
    B, S, D = xn.shape
    z = xn @ w_in
    u_pre, v_pre, q, k, v = jnp.split(
        z, [SGU_WIDTH, 2 * SGU_WIDTH, 2 * SGU_WIDTH + ATT_WIDTH, 2 * SGU_WIDTH + 2 * ATT_WIDTH], axis=-1)
    y_a = chunked_sgu(u_pre, v_pre, sgu_norm, w_spatial, b_spatial) @ w_branch_a
    att = moba_attention(q.reshape(B, S, ATT_HEADS, ATT_HEAD_DIM),
                         k.reshape(B, S, ATT_HEADS, ATT_HEAD_DIM),
                         v.reshape(B, S, ATT_HEADS, ATT_HEAD_DIM), rel_bias)
    y_b = att @ w_branch_b
    gates = jax.nn.sigmoid((xn @ w_gate).astype(jnp.float32) + b_gate.astype(jnp.float32))
    gates = gates.reshape(B, S, N_BRANCH, D).astype(xn.dtype)
    merged = gates[:, :, 0] * y_a + gates[:, :, 1] * y_b
    return merged @ w_o


def mem_cross_attention(xn, memn, w_xq, w_xkv, w_xo):
    B, S, _ = xn.shape
    M = memn.shape[1]
    q = (xn @ w_xq).reshape(B, S, X_HEADS, X_HEAD_DIM)
    kv = (memn @ w_xkv).reshape(B, M, 2, X_HEADS, X_HEAD_DIM)
    k, v = kv[:, :, 0], kv[:, :, 1]
    s = jnp.einsum('bqhd,bmhd->bhqm', q, k).astype(jnp.float32) * (X_HEAD_DIM ** -0.5)
    p = jax.nn.softmax(s, axis=-1).astype(v.dtype)
    o = jnp.einsum('bhqm,bmhd->bqhd', p, v).reshape(B, S, X_WIDTH)
    return o @ w_xo


def hier_moe(xn, w_rg, b_rg, w_re, b_re, w_e_gate, w_e_up, w_e_down):
    B, S, D = xn.shape
    T = B * S
    xt = xn.reshape(T, D)
    g_logits = (xt @ w_rg).astype(jnp.float32) + b_rg.astype(jnp.float32)
    p_g = jax.nn.softmax(g_logits, axis=-1)
    gsel = jnp.argmax(g_logits, axis=-1).astype(jnp.int32)
    pg_sel = jnp.take_along_axis(p_g, gsel[:, None], axis=-1)
    e_logits = ((xt @ w_re).astype(jnp.float32) + b_re.astype(jnp.float32)).reshape(T, N_GROUPS, EXPERTS_PER_GROUP)
    e_logits = jnp.take_along_axis(e_logits, gsel[:, None, None], axis=1)[:, 0]
    p_e = jax.nn.softmax(e_logits, axis=-1)
    top_p, top_e = lax.top_k(p_e, TOP_K_EXPERTS)
    top_p = top_p / jnp.sum(top_p, axis=-1, keepdims=True)
    weights = pg_sel * top_p
    expert_id = gsel[:, None] * EXPERTS_PER_GROUP + top_e
    P = T * TOP_K_EXPERTS
    eid = expert_id.reshape(P)
    tok = jnp.repeat(jnp.arange(T, dtype=jnp.int32), TOP_K_EXPERTS)
    wt = weights.reshape(P)
    order = jnp.argsort(eid)
    eid_s, tok_s, wt_s = eid[order], tok[order], wt[order]
    counts = jnp.bincount(eid, length=N_EXPERTS)
    starts = jnp.cumsum(counts) - counts
    padded = (counts + MOE_BLOCK - 1) // MOE_BLOCK * MOE_BLOCK
    pad_ends = jnp.cumsum(padded)
    pad_starts = pad_ends - padded
    dest = pad_starts[eid_s] + jnp.arange(P, dtype=jnp.int32) - starts[eid_s]
    NBLK = -(-P // MOE_BLOCK) + N_EXPERTS
    buf = jnp.zeros((NBLK * MOE_BLOCK, D), xt.dtype).at[dest].set(xt[tok_s])
    blk_expert = jnp.minimum(
        jnp.searchsorted(pad_ends, jnp.arange(NBLK, dtype=jnp.int32) * MOE_BLOCK, side='right'),
        N_EXPERTS - 1)

    def expert_block(a):
        xb, e = a
        hdn = jax.nn.silu(xb @ w_e_gate[e]) * (xb @ w_e_up[e])
        return hdn @ w_e_down[e]

    ybuf = lax.map(expert_block, (buf.reshape(NBLK, MOE_BLOCK, D), blk_expert)).reshape(NBLK * MOE_BLOCK, D)
    y = jnp.zeros((T, D), ybuf.dtype).at[tok_s].add(wt_s[:, None].astype(ybuf.dtype) * ybuf[dest])
    return y.reshape(B, S, D)


def setup_inputs(seed: int = 0) -> dict:
    key = jax.random.key(seed)
    ks = jax.random.split(key, 32)
    f32 = jnp.float32
    L = DEPTH
    D = D_MODEL

    def nrm(k, shape, scale):
        return jax.random.normal(k, shape, f32) * scale

    return {
        'x': nrm(ks[0], (BATCH, SEQ, D), 1.0),
        'mem': nrm(ks[1], (BATCH, MEM_LEN, D), 1.0),
        'norm_mix': 1.0 + nrm(ks[2], (L, D), 0.1),
        'w_in': nrm(ks[3], (L, D, IN_WIDTH), D ** -0.5),
        'w_gate': nrm(ks[4], (L, D, N_BRANCH * D), D ** -0.5),
        'b_gate': nrm(ks[5], (L, N_BRANCH * D), 0.1),
        'sgu_norm': 1.0 + nrm(ks[6], (L, SGU_WIDTH), 0.1),
        'w_spatial': nrm(ks[7], (L, SGU_GROUPS, CHUNK, CHUNK), CHUNK ** -0.5),
        'b_spatial': 1.0 + nrm(ks[8], (L, SGU_GROUPS, CHUNK), 0.1),
        'w_branch_a': nrm(ks[9], (L, SGU_WIDTH, D), SGU_WIDTH ** -0.5),
        'w_branch_b': nrm(ks[10], (L, ATT_WIDTH, D), ATT_WIDTH ** -0.5),
        'rel_bias': nrm(ks[11], (NUM_BUCKETS, ATT_HEADS), 0.3),
        'w_o': nrm(ks[12], (L, D, D), D ** -0.5),
        'norm_x': 1.0 + nrm(ks[13], (L, D), 0.1),
        'norm_mem': 1.0 + nrm(ks[14], (L, D), 0.1),
        'w_xq': nrm(ks[15], (L, D, X_WIDTH), D ** -0.5),
        'w_xkv': nrm(ks[16], (L, D, 2 * X_WIDTH), D ** -0.5),
        'w_xo': nrm(ks[17], (L, X_WIDTH, D), X_WIDTH ** -0.5),
        'norm_ffn': 1.0 + nrm(ks[18], (L, D), 0.1),
        'w_router_group': nrm(ks[19], (L, D, N_GROUPS), D ** -0.5),
        'b_router_group': nrm(ks[20], (L, N_GROUPS), 0.01),
        'w_router_expert': nrm(ks[21], (L, D, N_EXPERTS), D ** -0.5),
        'b_router_expert': nrm(ks[22], (L, N_EXPERTS), 0.01),
        'w_e_gate': nrm(ks[23], (L, N_EXPERTS, D, EXPERT_HIDDEN), D ** -0.5),
        'w_e_up': nrm(ks[24], (L, N_EXPERTS, D, EXPERT_HIDDEN), D ** -0.5),
        'w_e_down': nrm(ks[25], (L, N_EXPERTS, EXPERT_HIDDEN, D), EXPERT_HIDDEN ** -0.5),
        'norm_final': 1.0 + nrm(ks[26], (D,), 0.1),
    }


def reference(x, mem, norm_mix, w_in, w_gate, b_gate, sgu_norm, w_spatial, b_spatial,
              w_branch_a, w_branch_b, rel_bias, w_o, norm_x, norm_mem, w_xq, w_xkv, w_xo,
              norm_ffn, w_router_group, b_router_group, w_router_expert, b_router_expert,
              w_e_gate, w_e_up, w_e_down, norm_final):
    h = x
    for l in range(DEPTH):
        h = h + token_mixer(rmsnorm(h, norm_mix[l]), w_in[l], w_gate[l], b_gate[l], sgu_norm[l],
                            w_spatial[l], b_spatial[l], w_branch_a[l], w_branch_b[l], rel_bias, w_o[l])
        h = h + mem_cross_attention(rmsnorm(h, norm_x[l]), rmsnorm(mem, norm_mem[l]),
                                    w_xq[l], w_xkv[l], w_xo[l])
        h = h + hier_moe(rmsnorm(h, norm_ffn[l]), w_router_group[l], b_router_group[l],
                         w_router_expert[l], b_router_expert[l], w_e_gate[l], w_e_up[l], w_e_down[l])
    return rmsnorm(h, norm_final)
```

```python
import math
from contextlib import ExitStack

import numpy as np
import concourse.bass as bass
import concourse.mybir as mybir
from concourse.bass_utils import run_bass_kernel_spmd

F32 = mybir.dt.float32
BF16 = mybir.dt.bfloat16
I32 = mybir.dt.int32
AF = mybir.ActivationFunctionType
ALU = mybir.AluOpType
AX = mybir.AxisListType

S_LEN = 4096
D = 1024
NT = S_LEN // 128
NEXP = 64
CAP = 256
NSLOT = NEXP * CAP
NEGM = -30000.0


class Buf:
    __slots__ = ("lw", "rd")

    def __init__(self):
        self.lw = None
        self.rd = []


class Sched:
    ENG = ("pe", "act", "dve", "pool", "sp")
    SAME_ENGINE_SYNC = True

    def __init__(self, nc, semh):
        self.nc = nc
        self.semh = semh
        self.cnt = {k: 0 for k in semh}
        self.streams = {e: [] for e in self.ENG}
        self.waited = {e: {} for e in self.ENG}
        self.pools = {}
        self.regs = {}
        self.rr = {}
        for k in semh:
            if "_" in k:
                base = k.rsplit("_", 1)[0]
                self.pools.setdefault(base, []).append(k)
                self.rr[base] = 0

    def op(self, eng, fn, r=(), w=(), sem=None, inc=1, silent=False):
        dma = sem is not None
        if dma:
            pool = self.pools[sem]
            sem = pool[self.rr[sem] % len(pool)]
            self.rr[sem.rsplit("_", 1)[0]] += 1
        else:
            sem = eng
        waits = {}

        def need(ev):
            if ev is None:
                return
            s, v = ev
            if v > waits.get(s, 0):
                waits[s] = v

        for b in r:
            need(b.lw)
        for b in w:
            need(b.lw)
            for ev in b.rd:
                need(ev)
        if dma and self.cnt[sem] > 0:
            need((sem, self.cnt[sem]))
        myv = self.cnt[sem] + inc
        if not silent:
            self.cnt[sem] = myv
        ev = (sem, myv)
        for b in r:
            b.rd.append(ev)
        for b in w:
            b.lw = ev
            b.rd = []
        wd = self.waited[eng]
        wl = []
        for s, v in waits.items():
            if s == eng and (eng == "pe" or not self.SAME_ENGINE_SYNC):
                continue
            if s == sem and v >= myv:
                continue
            if wd.get(s, 0) >= v:
                continue
            wd[s] = v
            wl.append((s, v))
        self.streams[eng].append((wl, fn, sem, inc, silent))

    def barrier(self):
        snap = dict(self.cnt)
        for eng in self.ENG:
            wd = self.waited[eng]
            wl = []
            for s, v in snap.items():
                if v > 0 and wd.get(s, 0) < v:
                    wd[s] = v
                    wl.append((s, v))
            if wl:
                self.streams[eng].append((wl, None, None, 0, True))

    def flush(self, block):
        decos = {"pe": block.tensor, "act": block.scalar, "dve": block.vector,
                 "pool": block.gpsimd, "sp": block.sync}
        for name in self.ENG:
            ops = self.streams[name]
            final = dict(self.cnt) if name == "sp" else None

            def body(e, ops=ops, final=final, name=name):
                if name == "pool":
                    breg = e.alloc_register("bnd")
                    e.reg_mov(breg, NSLOT - 1)
                    self.regs["bnd"] = breg
                for wl, fn, sem, inc, silent in ops:
                    attach = (fn is not None and bool(wl) and name in ("act", "dve", "pool") and sem == name)
                    for s, v in (wl[:-1] if attach else wl):
                        e.wait_ge(self.semh[s], v)
                    if fn is None:
                        continue
                    ins = fn(e)
                    if attach:
                        ins._wait_ge(self.semh[wl[-1][0]], wl[-1][1])
                    if not silent:
                        ins.then_inc(self.semh[sem], inc)
                if final is not None:
                    for s, v in final.items():
                        if v > 0:
                            e.wait_ge(self.semh[s], v)

            decos[name](body)


class T:
    __slots__ = ("t", "b")

    def __init__(self, t):
        self.t = t
        self.b = Buf()

    def __getitem__(self, k):
        return self.t[k]


DMA_POOLS = {"dx": 4, "dw": 8, "dst": 8, "dps": 8, "dq": 6, "dg": 4, "dz": 4, "dc": 8, "dcw": 8}
SEM_NAMES = ["pe", "act", "dve", "pool", "sp"] + ["%s_%d" % (k, i) for k, n in DMA_POOLS.items() for i in range(n)]


def rel_bucket_np(dist):
    n = np.maximum(dist, 0)
    max_exact = 16
    nf = np.maximum(n, 1).astype(np.float32)
    large = max_exact + (np.log(nf / max_exact) / math.log(128 / max_exact) * (32 - max_exact)).astype(np.int32)
    large = np.minimum(large, 31)
    return np.where(n < max_exact, n, large)


def host_consts():
    c = {}
    k = np.arange(128)[:, None]
    q = np.arange(128)[None, :]
    oh = np.zeros((32, 2, 128, 128), np.float32)
    bd = rel_bucket_np(q - k)
    bp = rel_bucket_np(q + 128 - k)
    for r in range(32):
        oh[r, 0] = ((bd == r) & (q >= k))
        oh[r, 1] = (bp == r)
    c["c_oh"] = oh.reshape(32, 2 * 128 * 128)
    c["c_negT"] = np.where(q < k, NEGM, 0.0).astype(np.float32)
    c["c_tri"] = (k <= q).astype(np.float32)
    c["c_ltri"] = (k < q).astype(np.float32)
    pm = np.zeros((16, 16), np.float32)
    ps = np.zeros((16, 16), np.float32)
    for cc in range(16):
        pm[cc, cc:] = -1e30
        pm[cc, cc] = 1e30
        ps[cc, :cc] = 1.0
    c["c_pm"] = np.broadcast_to(pm.reshape(1, 256), (128, 256)).copy()
    c["c_psel"] = np.broadcast_to(ps.reshape(1, 256), (128, 256)).copy()
    es = np.zeros((16, 16, 128), np.float32)
    for n in range(16):
        es[n, n, :] = 1.0
    c["c_esel"] = es.reshape(16, 16 * 128)
    c["c_ebase"] = np.broadcast_to((np.arange(64) * CAP).astype(np.float32)[None, :], (128, 64)).copy()
    return c


def build(debug=False):
    nc = bass.Bass("TRN2", target_bir_lowering=False)

    def din(name, shape, dt=F32):
        return nc.dram_tensor(name, list(shape), dt, kind="ExternalInput").ap()

    def dscr(name, shape, dt):
        return nc.dram_tensor(name, list(shape), dt, kind=("ExternalOutput" if debug else "Internal")).ap()

    x_d = din("x", [S_LEN, D])
    mem_d = din("mem", [256, D])
    norm_mix_d = din("norm_mix", [D])
    w_in_d = din("w_in", [D, 5120])
    w_gate_d = din("w_gate", [D, 2048])
    b_gate_d = din("b_gate", [2048])
    sgu_norm_d = din("sgu_norm", [D])
    w_sp_d = din("w_spatial", [8, 128, 128])
    b_sp_d = din("b_spatial", [8, 128])
    w_a_d = din("w_branch_a", [D, D])
    w_b_d = din("w_branch_b", [D, D])
    relb_d = din("rel_bias", [32, 8])
    w_o_d = din("w_o", [D, D])
    norm_x_d = din("norm_x", [D])
    norm_mem_d = din("norm_mem", [D])
    w_xq_d = din("w_xq", [D, 512])
    w_xkv_d = din("w_xkv", [D, 1024])
    w_xo_d = din("w_xo", [512, D])
    norm_ffn_d = din("norm_ffn", [D])
    w_rg_d = din("w_router_group", [D, 8])
    b_rg_d = din("b_router_group", [8])
    w_re_d = din("w_router_expert", [D, 64])
    b_re_d = din("b_router_expert", [64])
    w_eg_d = din("w_e_gate", [NEXP, D, 512])
    w_eu_d = din("w_e_up", [NEXP, D, 512])
    w_ed_d = din("w_e_down", [NEXP, 512, D])
    norm_fin_d = din("norm_final", [D])
    c_oh_d = din("c_oh", [32, 32768])
    c_negT_d = din("c_negT", [128, 128])
    c_tri_d = din("c_tri", [128, 128])
    c_ltri_d = din("c_ltri", [128, 128])
    c_pm_d = din("c_pm", [128, 256])
    c_psel_d = din("c_psel", [128, 256])
    c_esel_d = din("c_esel", [16, 2048])
    c_ebase_d = din("c_ebase", [128, 64])

    out_d = nc.dram_tensor("out", [S_LEN, D], F32, kind="ExternalOutput").ap()

    xnT_d = dscr("xnT_s", [8, 128, S_LEN], BF16)
    ga_d = dscr("ga_s", [S_LEN, D], BF16)
    g1_d = dscr("g1_s", [S_LEN, D], BF16)
    qT_d = dscr("qT_s", [8, 128, S_LEN], BF16)
    kT_d = dscr("kT_s", [8, 128, S_LEN], BF16)
    v_d = dscr("v_s", [8, 128, NT, 129], BF16)
    bt_d = dscr("bt_s", [8, 2, 128, 128], F32)
    h2_d = dscr("h2_s", [S_LEN, D], F32)
    xbuf_d = dscr("xbuf_s", [NSLOT, D], BF16)
    wgb_d = nc.dram_tensor("wgb_s", [NEXP, 128, 8 * 512], BF16, kind="Internal").ap()
    wub_d = nc.dram_tensor("wub_s", [NEXP, 128, 8 * 512], BF16, kind="Internal").ap()
    wdb_d = nc.dram_tensor("wdb_s", [NEXP, 128, 4 * 1024], BF16, kind="Internal").ap()
    ybuf_d = dscr("ybuf_s", [NSLOT, D], BF16)
    att_d = dscr("att_s", [8, 128, S_LEN], BF16)
    dbg_rt = dscr("rt_s", [128, NT, 4], F32) if debug else None

    with ExitStack() as es:
        semh = {n: es.enter_context(nc.semaphore(n)) for n in SEM_NAMES}
        S = Sched(nc, semh)

        uid = [0]

        def sb(stack, name, shape, dt):
            uid[0] += 1
            return T(stack.enter_context(nc.sbuf_tensor("s%d_%s" % (uid[0], name), list(shape), dt)))

        def DMA(eng, out, in_, r, w, sem):
            S.op(eng, lambda e: e.dma_start(out=out, in_=in_), r=r, w=w, sem=sem, inc=16)

        def mm_acc(out_ap, pairs, r, w):
            n = len(pairs)
            for i, (l, rh) in enumerate(pairs):
                S.op("pe", lambda e, l=l, rh=rh, i=i: e.matmul(out_ap, lhsT=l, rhs=rh, start=(i == 0), stop=(i == n - 1)),
                     r=r, w=w, silent=(i < n - 1))

        cast_list = []
        for ex_ in range(NEXP):
            cast_list += [(wgb_d[ex_].rearrange("p (c n) -> p c n", c=8), w_eg_d[ex_].rearrange("(c p) n -> p c n", p=128)),
                          (wub_d[ex_].rearrange("p (c n) -> p c n", c=8), w_eu_d[ex_].rearrange("(c p) n -> p c n", p=128)),
                          (wdb_d[ex_].rearrange("p (c n) -> p c n", c=4), w_ed_d[ex_].rearrange("(c p) n -> p c n", p=128))]
        cast_pos = [0]

        def emit_casts(k):
            for _ in range(k):
                if cast_pos[0] < len(cast_list):
                    o_, i_ = cast_list[cast_pos[0]]
                    cast_pos[0] += 1
                    DMA("pool", o_, i_, [], [], "dcw")

        def ACTF(out, in_, func, r, w, **kw):
            S.op("act", lambda e: e.activation(out=out, in_=in_, func=func, **kw), r=r, w=w)

        def ACTC(out, in_, r, w):
            S.op("act", lambda e: e.copy(out=out, in_=in_), r=r, w=w)

        def TT(out, in0, in1, op, r, w, eng="dve"):
            S.op(eng, lambda e: e.tensor_tensor(out=out, in0=in0, in1=in1, op=op), r=r, w=w)

        def TS(out, in0, s1, s2, op0, op1, r, w, eng="dve"):
            if op1 is None:
                S.op(eng, lambda e: e.tensor_scalar(out=out, in0=in0, scalar1=s1, scalar2=None, op0=op0), r=r, w=w)
            else:
                S.op(eng, lambda e: e.tensor_scalar(out=out, in0=in0, scalar1=s1, scalar2=s2, op0=op0, op1=op1), r=r, w=w)

        def STT(out, in0, scalar, in1, op0, op1, r, w, eng="dve"):
            S.op(eng, lambda e: e.scalar_tensor_tensor(out=out, in0=in0, scalar=scalar, in1=in1, op0=op0, op1=op1), r=r, w=w)

        def CP(out, in_, r, w, eng="dve"):
            S.op(eng, lambda e: e.tensor_copy(out=out, in_=in_), r=r, w=w)

        def RED(out, in_, op, r, w):
            S.op("dve", lambda e: e.tensor_reduce(out=out, in_=in_, axis=AX.X, op=op), r=r, w=w)

        def RCP(out, in_, r, w):
            S.op("dve", lambda e: e.reciprocal(out=out, in_=in_), r=r, w=w)

        def MM(out, lhsT, rhs, r, w, start=True, stop=True, silent=False, skip=False):
            if skip:
                S.op("pe", lambda e: e.matmul(out, lhsT=lhsT, rhs=rhs, start=start, stop=stop, skip_group_check=True),
                     r=r, w=w, silent=silent)
            else:
                S.op("pe", lambda e: e.matmul(out, lhsT=lhsT, rhs=rhs, start=start, stop=stop), r=r, w=w, silent=silent)

        def TR(out, in_, idn, r, w, silent=False):
            S.op("pe", lambda e: e.transpose(out=out, in_=in_, identity=idn), r=r, w=w, silent=silent)

        def MSET(t_ap, val, w, eng="pool"):
            S.op(eng, lambda e: e.memset(t_ap, val), w=w)

        PS = [T(es.enter_context(nc.psum_tensor("ps%d" % i, [128, 512], F32))) for i in range(8)]

        def psb(i):
            return PS[i].t[:].bitcast(BF16)

        identf = sb(es, "identf", [128, 128], F32)
        ident = sb(es, "ident", [128, 128], BF16)
        onesb = sb(es, "onesb", [128, 128], BF16)
        slots = sb(es, "slots", [128, NT * 2], I32)
        wts = sb(es, "wts", [128, NT, 2], F32)
        gfin = sb(es, "gfin", [128, D], F32)
        xn3b = [sb(es, "xn3b%d" % i, [128, D], BF16) for i in range(2)]
        y1 = [sb(es, "y1_%d" % i, [128, D], BF16) for i in range(2)]
        y2 = [sb(es, "y2_%d" % i, [128, D], BF16) for i in range(2)]

        S.op("pool", lambda e: e.memset(identf[:], 0.0), w=[identf.b])
        S.op("pool", lambda e: e.affine_select(out=identf[:], in_=identf[:], pattern=[[-1, 128]], compare_op=ALU.not_equal,
                                                fill=1.0, base=0, channel_multiplier=1), r=[identf.b], w=[identf.b])
        S.op("dve", lambda e: e.tensor_copy(out=ident[:], in_=identf[:]), r=[identf.b], w=[ident.b])
        S.op("dve", lambda e: e.memset(onesb[:], 1.0), w=[onesb.b])
        DMA("sp", gfin[:], norm_fin_d.partition_broadcast(128), [], [gfin.b], "dc")

        def rmsnorm_rstd(src, ss, rstd, junk):
            S.op("act", lambda e: e.activation(out=junk[:], in_=src[:], func=AF.Square, accum_out=ss[:]),
                 r=[src.b], w=[junk.b, ss.b])
            S.op("act", lambda e: e.activation(out=rstd[:], in_=ss[:], func=AF.Sqrt, bias=1e-6, scale=1.0 / D),
                 r=[ss.b], w=[rstd.b])
            S.op("dve", lambda e: e.reciprocal(out=rstd[:], in_=rstd[:]), r=[rstd.b], w=[rstd.b])

        def transpose8(src_bf, dstT, bank, n=8, evac="act"):
            pv = psb(bank)
            for c in range(n):
                S.op("pe", lambda e, c=c: e.transpose(out=pv[:, c * 128:(c + 1) * 128], in_=src_bf[:, c * 128:(c + 1) * 128],
                                                      identity=ident[:]),
                     r=[src_bf.b, ident.b], w=[PS[bank].b], silent=(c < n - 1))
            if evac == "act":
                S.op("act", lambda e: e.copy(out=dstT[:].rearrange("p c t -> p (c t)"), in_=pv[:, 0:n * 128]),
                     r=[PS[bank].b], w=[dstT.b])
            else:
                S.op("dve", lambda e: e.tensor_copy(out=dstT[:].rearrange("p c t -> p (c t)"), in_=pv[:, 0:n * 128]),
                     r=[PS[bank].b], w=[dstT.b])

        with ExitStack() as p1:
            w_uv = sb(p1, "w_uv", [128, 8, 2048], BF16)
            w_g = sb(p1, "w_g", [128, 8, 2048], BF16)
            w_a = sb(p1, "w_a", [128, 8, 1024], BF16)
            wspT = sb(p1, "wspT", [128, 8, 128], BF16)
            wsp_f = sb(p1, "wsp_f", [128, 8, 128], F32)
            tri = sb(p1, "tri", [128, 128], F32)
            bspT = sb(p1, "bspT", [128, 8], F32)
            bsp_f = sb(p1, "bsp_f", [8, 128], F32)
            gmix = sb(p1, "gmix", [128, D], F32)
            gsgu = sb(p1, "gsgu", [128, D], F32)
            bgate = sb(p1, "bgate", [128, 2048], F32)
            DMA("pool", w_uv[:], w_in_d[:, 0:2048].rearrange("(c p) n -> p c n", p=128), [], [w_uv.b], "dw")
            DMA("pool", w_g[:], w_gate_d.rearrange("(c p) n -> p c n", p=128), [], [w_g.b], "dw")
            DMA("pool", w_a[:], w_a_d.rearrange("(c p) n -> p c n", p=128), [], [w_a.b], "dw")
            DMA("sp", wsp_f[:], w_sp_d.rearrange("g t s -> t g s"), [], [wsp_f.b], "dc")
            DMA("sp", tri[:], c_tri_d, [], [tri.b], "dc")
            DMA("sp", bsp_f[:], b_sp_d, [], [bsp_f.b], "dc")
            DMA("sp", gmix[:], norm_mix_d.partition_broadcast(128), [], [gmix.b], "dc")
            DMA("sp", gsgu[:], sgu_norm_d.partition_broadcast(128), [], [gsgu.b], "dc")
            DMA("sp", bgate[:], b_gate_d.partition_broadcast(128), [], [bgate.b], "dc")
            for g in range(8):
                S.op("pe", lambda e, g=g: e.transpose(out=PS[0][:, g * 128:(g + 1) * 128] if g < 4 else PS[1][:, (g - 4) * 128:(g - 3) * 128],
                                                      in_=wsp_f[:, g, :], identity=identf[:]),
                     r=[wsp_f.b, identf.b], w=[PS[0].b if g < 4 else PS[1].b])
            for g in range(8):
                src = PS[0] if g < 4 else PS[1]
                gg = g % 4
                S.op("dve", lambda e, g=g, src=src, gg=gg: e.tensor_tensor(out=wspT[:, g, :], in0=src[:, gg * 128:(gg + 1) * 128],
                                                                          in1=tri[:], op=ALU.mult),
                     r=[src.b, tri.b], w=[wspT.b])
            S.op("pe", lambda e: e.transpose(out=PS[2][:, 0:8], in_=bsp_f[:, :], identity=identf[0:8, 0:8]),
                 r=[bsp_f.b, identf.b], w=[PS[2].b])
            S.op("dve", lambda e: e.tensor_copy(out=bspT[:], in_=PS[2][:, 0:8]), r=[PS[2].b], w=[bspT.b])

            NB = 2
            xt = [sb(p1, "xt%d" % i, [128, D], F32) for i in range(NB)]
            junk = sb(p1, "junk1", [128, D], F32)
            ss = [sb(p1, "ss%d" % i, [128, 1], F32) for i in range(NB)]
            rstd = [sb(p1, "rstd%d" % i, [128, 1], F32) for i in range(NB)]
            xn = [sb(p1, "xn%d" % i, [128, D], BF16) for i in range(NB)]
            xnT = [sb(p1, "xnT%d" % i, [128, 8, 128], BF16) for i in range(NB)]
            sq = sb(p1, "sq", [128, 2048], F32)
            inner = sb(p1, "inner", [128, 2048], F32)
            sg_ = sb(p1, "sgm", [128, 2048], F32)
            uv = sb(p1, "uv", [128, 2048], F32)
            ssv = sb(p1, "ssv", [128, 1], F32)
            rstdv = sb(p1, "rstdv", [128, 1], F32)
            vn = sb(p1, "vn", [128, D], BF16)
            sgb = sb(p1, "sgb", [128, D], BF16)
            sgT = sb(p1, "sgT", [128, 8, 128], BF16)
            gpre = sb(p1, "gpre", [128, 2048], F32)
            gts = sb(p1, "gts", [128, 2048], F32)
            ga_t = [sb(p1, "ga_t%d" % i, [128, D], BF16) for i in range(NB)]
            g1_t = [sb(p1, "g1_t%d" % i, [128, D], BF16) for i in range(NB)]

            junkh = sb(p1, "junkh", [128, D], F32)

            def head1(i):
                k = i % NB
                rows = slice(i * 128, (i + 1) * 128)
                DMA("sp", xt[k][:], x_d[rows, :], [], [xt[k].b], "dx")
                rmsnorm_rstd(xt[k], ss[k], rstd[k], junkh)
                STT(xn[k][:], xt[k][:], rstd[k][:], gmix[:], ALU.mult, ALU.mult, [xt[k].b, rstd[k].b, gmix.b], [xn[k].b])
                transpose8(xn[k], xnT[k], 0)
                DMA("pool", xnT_d[:, :, rows].rearrange("c p t -> p c t"), xnT[k][:], [xnT[k].b], [], "dps")

            head1(0)
            for i in range(NT):
                k = i % NB
                rows = slice(i * 128, (i + 1) * 128)
                emit_casts(2)
                for nb in range(4):
                    mm_acc(PS[nb][:], [(xnT[k][:, c, :], w_uv[:, c, nb * 512:(nb + 1) * 512]) for c in range(8)],
                           [xnT[k].b, w_uv.b], [PS[nb].b])
                for nb in range(4):
                    mm_acc(PS[4 + nb][:], [(xnT[k][:, c, :], w_g[:, c, nb * 512:(nb + 1) * 512]) for c in range(8)],
                           [xnT[k].b, w_g.b], [PS[4 + nb].b])
                for nb in range(4):
                    cs = slice(nb * 512, (nb + 1) * 512)
                    S.op("act", lambda e, nb=nb, cs=cs: e.activation(out=sq[:, cs], in_=PS[nb][:], func=AF.Square),
                         r=[PS[nb].b], w=[sq.b])
                    S.op("dve", lambda e, nb=nb, cs=cs: e.scalar_tensor_tensor(out=inner[:, cs], in0=sq[:, cs], scalar=0.044715,
                                                                               in1=PS[nb][:], op0=ALU.mult, op1=ALU.mult),
                         r=[sq.b, PS[nb].b], w=[inner.b])
                    S.op("dve", lambda e, nb=nb, cs=cs: e.tensor_tensor(out=inner[:, cs], in0=inner[:, cs], in1=PS[nb][:], op=ALU.add),
                         r=[inner.b, PS[nb].b], w=[inner.b])
                S.op("act", lambda e: e.activation(out=sg_[:], in_=inner[:], func=AF.Sigmoid, scale=1.5957691216),
                     r=[inner.b], w=[sg_.b])
                for nb in range(4):
                    cs = slice(nb * 512, (nb + 1) * 512)
                    S.op("dve", lambda e, nb=nb, cs=cs: e.tensor_tensor(out=uv[:, cs], in0=sg_[:, cs], in1=PS[nb][:], op=ALU.mult),
                         r=[sg_.b, PS[nb].b], w=[uv.b])
                S.op("act", lambda e: e.activation(out=junk[:], in_=uv[:, 1024:2048], func=AF.Square, accum_out=ssv[:]),
                     r=[uv.b], w=[junk.b, ssv.b])
                S.op("act", lambda e: e.activation(out=rstdv[:], in_=ssv[:], func=AF.Sqrt, bias=1e-6, scale=1.0 / D),
                     r=[ssv.b], w=[rstdv.b])
                S.op("dve", lambda e: e.reciprocal(out=rstdv[:], in_=rstdv[:]), r=[rstdv.b], w=[rstdv.b])
                S.op("dve", lambda e: e.scalar_tensor_tensor(out=vn[:], in0=uv[:, 1024:2048], scalar=rstdv[:], in1=gsgu[:],
                                                             op0=ALU.mult, op1=ALU.mult),
                     r=[uv.b, rstdv.b, gsgu.b], w=[vn.b])
                for g in range(8):
                    bk = g // 4
                    gg = g % 4
                    S.op("pe", lambda e, g=g, bk=bk, gg=gg: e.matmul(PS[bk][:, gg * 128:(gg + 1) * 128], lhsT=wspT[:, g, :],
                                                                     rhs=vn[:, g * 128:(g + 1) * 128], start=True, stop=True),
                         r=[wspT.b, vn.b], w=[PS[bk].b], silent=(gg < 3))
                for g in range(8):
                    bk = g // 4
                    gg = g % 4
                    S.op("dve", lambda e, g=g, bk=bk, gg=gg: e.scalar_tensor_tensor(
                        out=sgb[:, g * 128:(g + 1) * 128], in0=PS[bk][:, gg * 128:(gg + 1) * 128], scalar=bspT[:, g:g + 1],
                        in1=uv[:, g * 128:(g + 1) * 128], op0=ALU.add, op1=ALU.mult),
                        r=[PS[bk].b, bspT.b, uv.b], w=[sgb.b])
                transpose8(sgb, sgT, 2)
                if i + 1 < NT:
                    head1(i + 1)
                for nb in range(4):
                    cs = slice(nb * 512, (nb + 1) * 512)
                    S.op("dve", lambda e, nb=nb, cs=cs: e.tensor_tensor(out=gpre[:, cs], in0=PS[4 + nb][:], in1=bgate[:, cs], op=ALU.add),
                         r=[PS[4 + nb].b, bgate.b], w=[gpre.b])
                S.op("act", lambda e: e.activation(out=gts[:], in_=gpre[:], func=AF.Sigmoid), r=[gpre.b], w=[gts.b])
                for hf in range(2):
                    mm_acc(PS[2 + hf][:], [(sgT[:, c, :], w_a[:, c, hf * 512:(hf + 1) * 512]) for c in range(8)],
                           [sgT.b, w_a.b], [PS[2 + hf].b])
                for hf in range(2):
                    cs = slice(hf * 512, (hf + 1) * 512)
                    S.op("dve", lambda e, hf=hf, cs=cs, k=k: e.tensor_tensor(out=ga_t[k][:, cs], in0=PS[2 + hf][:], in1=gts[:, cs], op=ALU.mult),
                         r=[PS[2 + hf].b, gts.b], w=[ga_t[k].b])
                S.op("act", lambda e, k=k: e.copy(out=g1_t[k][:], in_=gts[:, 1024:2048]), r=[gts.b], w=[g1_t[k].b])
                DMA("pool", ga_d[rows, :], ga_t[k][:], [ga_t[k].b], [], "dps")
                DMA("pool", g1_d[rows, :], g1_t[k][:], [g1_t[k].b], [], "dps")
            S.barrier()


        SCALE = 1.0 / math.sqrt(128.0)
        kmsum = sb(es, "kmsum", [128, 8, 16], F32)
        pA = ExitStack()
        attT = sb(pA, "attT", [128, 8, S_LEN], BF16)
        attTb = [Buf() for _ in range(NT)]
        with ExitStack() as p2:
            w_q = sb(p2, "w_q", [128, 8, 1024], BF16)
            w_k = sb(p2, "w_k", [128, 8, 1024], BF16)
            w_v = sb(p2, "w_v", [128, 8, 1024], BF16)
            DMA("pool", w_q[:], w_in_d[:, 2048:3072].rearrange("(c p) n -> p c n", p=128), [], [w_q.b], "dw")
            DMA("pool", w_k[:], w_in_d[:, 3072:4096].rearrange("(c p) n -> p c n", p=128), [], [w_k.b], "dw")
            DMA("pool", w_v[:], w_in_d[:, 4096:5120].rearrange("(c p) n -> p c n", p=128), [], [w_v.b], "dw")
            xg = [sb(p2, "xg%d" % i, [128, 8, 512], BF16) for i in range(2)]
            qsb = [sb(p2, "qsb%d" % i, [128, 512], BF16) for i in range(3)]
            ksb = [sb(p2, "ksb%d" % i, [128, 512], BF16) for i in range(3)]
            vaug = [sb(p2, "vaug%d" % i, [128, 8, 129], BF16) for i in range(2)]
            for i in range(2):
                S.op("pool", lambda e, i=i: e.memset(vaug[i][:, :, 128:129], 1.0), w=[vaug[i].b])
            cnt = 0
            for tg in range(8):
                k = tg % 2
                cols = slice(tg * 512, (tg + 1) * 512)
                DMA("sp", xg[k][:], xnT_d[:, :, cols].rearrange("c p t -> p c t"), [], [xg[k].b], "dx")
                for h in range(8):
                    j = cnt % 3
                    cnt += 1
                    bq = (2 * h) % 4
                    bk = (2 * h + 1) % 4
                    hs = slice(h * 128, (h + 1) * 128)
                    mm_acc(PS[bq][:], [(w_q[:, c, hs], xg[k][:, c, :]) for c in range(8)], [w_q.b, xg[k].b], [PS[bq].b])
                    S.op("act", lambda e, j=j, bq=bq: e.activation(out=qsb[j][:], in_=PS[bq][:], func=AF.Copy, scale=SCALE),
                         r=[PS[bq].b], w=[qsb[j].b])
                    DMA("pool", qT_d[h, :, cols], qsb[j][:], [qsb[j].b], [], "dps")
                    mm_acc(PS[bk][:], [(w_k[:, c, hs], xg[k][:, c, :]) for c in range(8)], [w_k.b, xg[k].b], [PS[bk].b])
                    S.op("dve", lambda e, j=j, bk=bk: e.tensor_copy(out=ksb[j][:], in_=PS[bk][:]), r=[PS[bk].b], w=[ksb[j].b])
                    S.op("dve", lambda e, bk=bk, h=h, tg=tg: e.tensor_reduce(out=kmsum[:, h, tg * 2:(tg + 1) * 2],
                                                                             in_=PS[bk][:].rearrange("p (b t) -> p b t", b=2),
                                                                             axis=AX.X, op=ALU.add),
                         r=[PS[bk].b], w=[kmsum.b])
                    DMA("pool", kT_d[h, :, cols], ksb[j][:], [ksb[j].b], [], "dps")
                for tt in range(4):
                    i = tg * 4 + tt
                    jv = i % 2
                    for hf in range(2):
                        mm_acc(PS[4 + hf][:], [(xg[k][:, c, tt * 128:(tt + 1) * 128], w_v[:, c, hf * 512:(hf + 1) * 512]) for c in range(8)],
                               [xg[k].b, w_v.b], [PS[4 + hf].b])
                    S.op("act", lambda e, jv=jv: e.copy(out=vaug[jv][:, 0:4, 0:128], in_=PS[4][:].rearrange("p (h d) -> p h d", h=4)),
                         r=[PS[4].b], w=[vaug[jv].b])
                    S.op("dve", lambda e, jv=jv: e.tensor_copy(out=vaug[jv][:, 4:8, 0:128], in_=PS[5][:].rearrange("p (h d) -> p h d", h=4)),
                         r=[PS[5].b], w=[vaug[jv].b])
                    DMA("pool", v_d[:, :, i, :].rearrange("h p d -> p h d"), vaug[jv][:], [vaug[jv].b], [], "dps")
            S.barrier()

        with ExitStack() as p3:
            relb = sb(p3, "relb", [32, 8], F32)
            b31bc = sb(p3, "b31bc", [128, 8], F32)
            negT = sb(p3, "negT", [128, 128], F32)
            pm = sb(p3, "pm", [128, 256], F32)
            psel = sb(p3, "psel", [128, 256], F32)
            esel_f = sb(p3, "esel_f", [16, 2048], F32)
            esel = sb(p3, "esel", [16, 16, 128], BF16)
            kmT = sb(p3, "kmT", [128, 8, 16], BF16)
            BT = sb(p3, "BT", [128, 8, 2, 128], F32)
            BTb = sb(p3, "BTb", [128, 8, 2, 128], BF16)
            maskT = sb(p3, "maskT", [16, S_LEN], BF16)
            btd = Buf()
            DMA("sp", relb[:], relb_d, [], [relb.b], "dc")
            DMA("sp", b31bc[:], relb_d[31, :].partition_broadcast(128), [], [b31bc.b], "dc")
            DMA("sp", negT[:], c_negT_d, [], [negT.b], "dc")
            DMA("sp", pm[:], c_pm_d, [], [pm.b], "dc")
            DMA("sp", psel[:], c_psel_d, [], [psel.b], "dc")
            DMA("sp", esel_f[:], c_esel_d, [], [esel_f.b], "dc")
            S.op("dve", lambda e: e.tensor_copy(out=esel[:].rearrange("a n k -> a (n k)"), in_=esel_f[:]), r=[esel_f.b], w=[esel.b])
            S.op("dve", lambda e: e.tensor_scalar(out=kmT[:], in0=kmsum[:], scalar1=1.0 / 256, scalar2=None, op0=ALU.mult),
                 r=[kmsum.b], w=[kmT.b])
            with ExitStack() as pb:
                oh_sb = [sb(pb, "oh_sb%d" % i, [32, 4096], F32) for i in range(2)]
                btst = [sb(pb, "btst%d" % i, [8, 4096], F32) for i in range(2)]
                btflat = bt_d.rearrange("h w k q -> h (w k q)")
                for ch in range(8):
                    kk = ch % 2
                    DMA("sp", oh_sb[kk][:], c_oh_d[:, ch * 4096:(ch + 1) * 4096], [], [oh_sb[kk].b], "dx")
                    for s8 in range(8):
                        bank = s8 % 2
                        S.op("pe", lambda e, kk=kk, s8=s8, bank=bank: e.matmul(PS[bank][0:8, :], lhsT=relb[:, :],
                                                                               rhs=oh_sb[kk][:, s8 * 512:(s8 + 1) * 512], start=True, stop=True),
                             r=[relb.b, oh_sb[kk].b], w=[PS[bank].b])
                        S.op("act", lambda e, kk=kk, s8=s8, bank=bank: e.copy(out=btst[kk][:, s8 * 512:(s8 + 1) * 512], in_=PS[bank][0:8, :]),
                             r=[PS[bank].b], w=[btst[kk].b])
                    DMA("sp", btflat[:, ch * 4096:(ch + 1) * 4096], btst[kk][:], [btst[kk].b], [btd], "dst")
                DMA("sp", BT[:], bt_d.rearrange("h w k q -> k h w q"), [btd], [BT.b], "dx")
                for h in range(8):
                    S.op("dve", lambda e, h=h: e.tensor_scalar(out=BT[:, h, :, :].rearrange("p w q -> p (w q)"),
                                                               in0=BT[:, h, :, :].rearrange("p w q -> p (w q)"),
                                                               scalar1=b31bc[:, h:h + 1], scalar2=None, op0=ALU.subtract),
                         r=[BT.b, b31bc.b], w=[BT.b])
                    S.op("dve", lambda e, h=h: e.tensor_tensor(out=BT[:, h, 0, :], in0=BT[:, h, 0, :], in1=negT[:], op=ALU.add),
                         r=[BT.b, negT.b], w=[BT.b])
                CP(BTb[:].rearrange("p h w q -> p (h w q)"), BT[:].rearrange("p h w q -> p (h w q)"), [BT.b], [BTb.b])
                S.barrier()

            qh = [sb(p3, "qh%d" % i, [128, S_LEN], BF16) for i in range(2)]
            kh = [sb(p3, "kh%d" % i, [128, S_LEN], BF16) for i in range(2)]
            vh = [sb(p3, "vh%d" % i, [128, NT, 129], BF16) for i in range(2)]
            NR = 4
            gp_sb = [sb(p3, "gp_sb%d" % i, [128, 16], F32) for i in range(NR)]
            top8 = [sb(p3, "top8%d" % i, [128, 8], F32) for i in range(NR)]
            mb = [sb(p3, "mb%d" % i, [128, 16], F32) for i in range(NR)]
            PT = [sb(p3, "PT%d" % i, [128, 512], BF16) for i in range(4)]
            tmpS = [sb(p3, "tmpS%d" % i, [128, 256], F32) for i in range(3)]
            rc = [sb(p3, "rc%d" % i, [128, 1], F32) for i in range(4)]
            attb = [sb(p3, "attb%d" % i, [128, 128], BF16) for i in range(4)]

            def load_head(h):
                kk = h % 2
                DMA("sp", qh[kk][:], qT_d[h], [], [qh[kk].b], "dq")
                DMA("sp", kh[kk][:], kT_d[h], [], [kh[kk].b], "dq")
                DMA("sp", vh[kk][:], v_d[h], [], [vh[kk].b], "dq")

            class Reg:
                def __init__(self, bank, off):
                    self.ap = PS[bank][:, off:off + 129]
                    self.b = PS[bank].b
            class Reg2(Reg):
                def __init__(self, bank, off):
                    Reg.__init__(self, bank, off)
                    self.bank = bank
            blk = [[Reg2((2 if p_ == 0 else 4) + lt // 2, (lt % 2) * 129) for p_ in range(2)] for lt in range(4)]
            m_all = [sb(p3, "m_all%d" % i, [128, NT, 16], F32) for i in range(2)]
            acc = [sb(p3, "acc%d" % i, [128, 129], F32) for i in range(8)]

            SBK = (0, 1, 6)
            load_head(0)
            stepc = 0
            for h in range(8):
                kk = h % 2
                if h + 1 < 8:
                    load_head(h + 1)
                qT_, kT_, vv_ = qh[kk], kh[kk], vh[kk]
                mh_ = m_all[kk]
                def mask_tile(hh, i):
                    qTn, mhn = qh[hh % 2], m_all[hh % 2]
                    c = i // 2
                    rr_ = i % NR
                    gcol = (i % 8) * 16
                    MM(PS[7][:, gcol:gcol + 16], qTn[:, i * 128:(i + 1) * 128], kmT[:, hh, :], [qTn.b, kmT.b], [PS[7].b])
                    TT(gp_sb[rr_][:], PS[7][:, gcol:gcol + 16], pm[:, c * 16:(c + 1) * 16], ALU.add, [PS[7].b, pm.b], [gp_sb[rr_].b])
                    S.op("dve", lambda e, rr_=rr_: e.max(out=top8[rr_][:], in_=gp_sb[rr_][:]), r=[gp_sb[rr_].b], w=[top8[rr_].b])
                    TS(mhn[:, i, :], gp_sb[rr_][:], top8[rr_][:, 3:4], None, ALU.is_ge, None, [gp_sb[rr_].b, top8[rr_].b], [mhn.b])

                if h == 0:
                    for i in range(NT):
                        mask_tile(0, i)
                steps = [(g, j) for g in range(8) for j in range(4 * g + 4)]

                def emit_S(idx, kT_=kT_, qT_=qT_, h=h):
                    g, j = steps[idx]
                    r_ = j - 4 * g
                    q0 = max(r_, 0) * 128
                    sb_ = PS[SBK[(stepc + idx) % 3]]
                    if r_ >= 0:
                        s0, w_, b0 = q0, (256 if r_ <= 2 else 128), 0
                    elif r_ == -1:
                        s0, w_, b0 = 0, 128, 128
                    else:
                        s0, w_, b0 = 0, 0, 0
                    MM(sb_[:, q0:512], kT_[:, j * 128:(j + 1) * 128], qT_[:, g * 512 + q0:(g + 1) * 512], [kT_.b, qT_.b], [sb_.b],
                       start=True, stop=True, silent=(w_ > 0))
                    if w_ > 0:
                        MM(sb_[:, s0:s0 + w_], ident[:], BTb[:, h, :, :].rearrange("p w q -> p (w q)")[:, b0:b0 + w_],
                           [ident.b, BTb.b], [sb_.b], start=False, stop=True, skip=True)

                emit_S(0)
                emit_S(1)
                pending_tr = []
                for idx, (g, j) in enumerate(steps):
                    if idx + 2 < len(steps):
                        emit_S(idx + 2)
                    if j == 0:
                        emit_casts(2 if g < 4 else 1)
                    if h + 1 < 8 and idx % 4 == 0 and idx // 4 < NT:
                        mask_tile(h + 1, idx // 4)
                    r_ = j - 4 * g
                    n = j // 2
                    q0 = max(r_, 0) * 128
                    sb_ = PS[SBK[(stepc + idx) % 3]]
                    pt = PT[(stepc + idx) % 4]
                    ts_ = tmpS[(stepc + idx) % 3]
                    ACTF(pt[:, q0:512], sb_[:, q0:512], AF.Exp, [sb_.b], [pt.b])
                    started = set()
                    post = []
                    for lt in range(max(r_, 0), 4):
                        i = 4 * g + lt
                        ac = acc[(g % 2) * 4 + lt]
                        rg = blk[lt][n % 2]
                        sp_ = (j % 2 == 1) or (j == i)
                        if j % 2 == 0:
                            st_ = rg.bank not in started
                            started.add(rg.bank)
                        else:
                            st_ = False
                        MM(rg.ap, pt[:, lt * 128:(lt + 1) * 128], vv_[:, j, :], [pt.b, vv_.b], [rg.b], start=st_, stop=sp_,
                           silent=False, skip=True)
                        post.append((lt, i, ac, rg, sp_))
                    for lt, i, ac, rg, sp_ in post:
                        if sp_:
                            if n == 0:
                                TS(ac[:], rg.ap, mh_[:, i, 0:1], None, ALU.mult, None, [rg.b, mh_.b], [ac.b])
                            else:
                                STT(ac[:], rg.ap, mh_[:, i, n:n + 1], ac[:], ALU.mult, ALU.add, [rg.b, mh_.b, ac.b], [ac.b])
                    for fn_ in pending_tr:
                        fn_()
                    pending_tr = []
                    for lt, i, ac, rg, sp_ in post:
                        if j == i:
                            RCP(rc[lt][:], ac[:, 128:129], [ac.b], [rc[lt].b])
                            TS(attb[lt][:], ac[:, 0:128], rc[lt][:], None, ALU.mult, None, [ac.b, rc[lt].b], [attb[lt].b])

                            def fin_(lt=lt, i=i, h=h):
                                pv7 = psb(7)
                                TR(pv7[:, lt * 128:(lt + 1) * 128], attb[lt][:], ident[:], [attb[lt].b, ident.b], [PS[7].b])
                                ACTC(attT[:, h, i * 128:(i + 1) * 128], pv7[:, lt * 128:(lt + 1) * 128], [PS[7].b], [attTb[i]])
                            pending_tr.append(fin_)
                for fn_ in pending_tr:
                    fn_()
                pending_tr = []
                stepc += len(steps)
                DMA("pool", att_d[h], attT[:, h, :], list(attTb), [], "dps")
            S.barrier()
        pA.close()


        xbufb = Buf()
        with ExitStack() as pz:
            zt = sb(pz, "zt", [128, 4096], BF16)
            MSET(zt[:], 0.0, [zt.b], eng="dve")
            xv = xbuf_d.rearrange("(a p) d -> p a d", p=128)
            for a in range(32):
                DMA("sp", xv[:, a * 4:(a + 1) * 4, :], zt[:].rearrange("p (a d) -> p a d", a=4), [zt.b], [xbufb], "dz")
            S.barrier()

        with ExitStack() as p4:
            w_b = sb(p4, "w_b", [128, 8, 1024], BF16)
            w_o = sb(p4, "w_o", [128, 8, 1024], BF16)
            w_xq = sb(p4, "w_xq", [128, 8, 512], BF16)
            w_xo = sb(p4, "w_xo", [128, 4, 1024], BF16)
            w_r = sb(p4, "w_r", [128, 8, 72], F32)
            b_r = sb(p4, "b_r", [128, 72], F32)
            gx = sb(p4, "gx", [128, D], F32)
            gffn = sb(p4, "gffn", [128, D], F32)
            ebase = sb(p4, "ebase", [128, 64], F32)
            ltri = sb(p4, "ltri", [128, 128], BF16)
            cum = sb(p4, "cum", [128, 64], F32)
            memKT = sb(p4, "memKT", [128, 4, 256], BF16)
            memV = sb(p4, "memV", [128, 2, 4, 129], BF16)
            DMA("pool", w_b[:], w_b_d.rearrange("(c p) n -> p c n", p=128), [], [w_b.b], "dw")
            DMA("pool", w_o[:], w_o_d.rearrange("(c p) n -> p c n", p=128), [], [w_o.b], "dw")
            DMA("pool", w_xq[:], w_xq_d.rearrange("(c p) n -> p c n", p=128), [], [w_xq.b], "dw")
            DMA("pool", w_xo[:], w_xo_d.rearrange("(c p) n -> p c n", p=128), [], [w_xo.b], "dw")
            DMA("pool", ltri[:], c_ltri_d, [], [ltri.b], "dw")
            DMA("sp", w_r[:, :, 0:8], w_rg_d.rearrange("(c p) n -> p c n", p=128), [], [w_r.b], "dc")
            DMA("sp", w_r[:, :, 8:72], w_re_d.rearrange("(c p) n -> p c n", p=128), [], [w_r.b], "dc")
            DMA("sp", b_r[:, 0:8], b_rg_d.partition_broadcast(128), [], [b_r.b], "dc")
            DMA("sp", b_r[:, 8:72], b_re_d.partition_broadcast(128), [], [b_r.b], "dc")
            DMA("sp", gx[:], norm_x_d.partition_broadcast(128), [], [gx.b], "dc")
            DMA("sp", gffn[:], norm_ffn_d.partition_broadcast(128), [], [gffn.b], "dc")
            DMA("sp", ebase[:], c_ebase_d, [], [ebase.b], "dc")
            MSET(cum[:], 0.0, [cum.b], eng="dve")
            MSET(memV[:, :, :, 128:129], 1.0, [memV.b], eng="dve")

            junk3 = sb(p4, "junk3", [128, D], F32)
            ss3 = sb(p4, "ss3", [128, 1], F32)
            rstd3 = sb(p4, "rstd3", [128, 1], F32)
            with ExitStack() as pm_:
                w_xkv = sb(pm_, "w_xkv", [128, 8, 1024], BF16)
                gmem = sb(pm_, "gmem", [128, D], F32)
                DMA("pool", w_xkv[:], w_xkv_d.rearrange("(c p) n -> p c n", p=128), [], [w_xkv.b], "dw")
                DMA("sp", gmem[:], norm_mem_d.partition_broadcast(128), [], [gmem.b], "dc")
                memt = sb(pm_, "memt", [128, D], F32)
                memn = sb(pm_, "memn", [128, D], BF16)
                memnT = sb(pm_, "memnT", [128, 8, 256], BF16)
                mT1 = sb(pm_, "mT1", [128, 8, 128], BF16)
                for mh in range(2):
                    DMA("sp", memt[:], mem_d[mh * 128:(mh + 1) * 128, :], [], [memt.b], "dx")
                    rmsnorm_rstd(memt, ss3, rstd3, junk3)
                    STT(memn[:], memt[:], rstd3[:], gmem[:], ALU.mult, ALU.mult, [memt.b, rstd3.b, gmem.b], [memn.b])
                    transpose8(memn, mT1, 7)
                    CP(memnT[:, :, mh * 128:(mh + 1) * 128], mT1[:], [mT1.b], [memnT.b])
                for h4 in range(4):
                    mm_acc(PS[h4 % 2][:, 0:256], [(w_xkv[:, c, h4 * 128:(h4 + 1) * 128], memnT[:, c, :]) for c in range(8)],
                           [w_xkv.b, memnT.b], [PS[h4 % 2].b])
                    ACTC(memKT[:, h4, :], PS[h4 % 2][:, 0:256], [PS[h4 % 2].b], [memKT.b])
                for mh in range(2):
                    mm_acc(PS[2 + mh][:], [(memnT[:, c, mh * 128:(mh + 1) * 128], w_xkv[:, c, 512:1024]) for c in range(8)],
                           [w_xkv.b, memnT.b], [PS[2 + mh].b])
                    CP(memV[:, mh, :, 0:128], PS[2 + mh][:].rearrange("p (h d) -> p h d", h=4), [PS[2 + mh].b], [memV.b])
                S.barrier()

            class WK:
                pass
            wk = []
            for p_ in range(2):
                W = WK()
                for nm, shp, dt in [("gat", [128, D], BF16), ("g1t", [128, D], BF16), ("xt3", [128, D], F32), ("att", [128, 8, 128], BF16),
                                    ("mrg_f", [128, D], F32), ("mrg_b", [128, D], BF16), ("mT", [128, 8, 128], BF16), ("h1", [128, D], F32),
                                    ("xn2", [128, D], BF16), ("xn2T", [128, 8, 128], BF16), ("q2T", [128, 4, 128], BF16),
                                    ("P2T", [128, 8, 128], BF16), ("rc2", [128, 4], F32), ("ox", [128, 512], BF16), ("oxT", [128, 4, 128], BF16),
                                    ("h2", [128, D], F32), ("xn3f", [128, D], F32), ("xn3T", [128, 8, 128], F32), ("junk", [128, D], F32),
                                    ("ss", [128, 1], F32), ("rstd", [128, 1], F32),
                                    ("lg", [128, 72], F32), ("gmax", [128, 1], F32), ("ngmax", [128, 1], F32), ("ohg", [128, 8], F32),
                                    ("eg", [128, 8], F32), ("sumg", [128, 1], F32), ("pg", [128, 1], F32), ("pen", [128, 8], F32),
                                    ("em", [128, 64], F32), ("tp8", [128, 8], F32), ("sel1", [128, 64], F32), ("sel2", [128, 64], F32),
                                    ("selb", [128, 64], BF16), ("dd", [128, 1], F32), ("ee", [128, 1], F32), ("w1", [128, 1], F32),
                                    ("w2", [128, 1], F32), ("posf", [128, 64], F32), ("over", [128, 64], F32), ("prod", [128, 64], F32),
                                    ("slf", [128, 2], F32)]:
                    setattr(W, nm, sb(p4, "%s_%d" % (nm, p_), shp, dt))
                wk.append(W)

            def tile3(i):
                p_ = i % 2
                W = wk[p_]
                A, B_, C, TB = (0, 1, 2, 3) if p_ == 0 else (4, 5, 6, 7)
                AB = (A, B_)
                rows = slice(i * 128, (i + 1) * 128)
                emit_casts(1)
                DMA("sp", W.att[:], att_d[:, :, rows].rearrange("c p t -> p c t"), [], [W.att.b], "dx")
                DMA("sp", W.gat[:], ga_d[rows, :], [], [W.gat.b], "dx")
                DMA("sp", W.g1t[:], g1_d[rows, :], [], [W.g1t.b], "dx")
                DMA("sp", W.xt3[:], x_d[rows, :], [], [W.xt3.b], "dx")
                for hf, bk in enumerate(AB):
                    mm_acc(PS[bk][:], [(W.att[:, h, :], w_b[:, h, hf * 512:(hf + 1) * 512]) for h in range(8)],
                           [W.att.b, w_b.b], [PS[bk].b])
                yield
                for hf, bk in enumerate(AB):
                    cs = slice(hf * 512, (hf + 1) * 512)
                    TT(W.mrg_f[:, cs], PS[bk][:], W.g1t[:, cs], ALU.mult, [PS[bk].b, W.g1t.b], [W.mrg_f.b])
                TT(W.mrg_b[:], W.mrg_f[:], W.gat[:], ALU.add, [W.mrg_f.b, W.gat.b], [W.mrg_b.b])
                transpose8(W.mrg_b, W.mT, TB)
                yield
                for hf, bk in enumerate(AB):
                    mm_acc(PS[bk][:], [(W.mT[:, c, :], w_o[:, c, hf * 512:(hf + 1) * 512]) for c in range(8)],
                           [W.mT.b, w_o.b], [PS[bk].b])
                yield
                for hf, bk in enumerate(AB):
                    cs = slice(hf * 512, (hf + 1) * 512)
                    TT(W.h1[:, cs], PS[bk][:], W.xt3[:, cs], ALU.add, [PS[bk].b, W.xt3.b], [W.h1.b])
                rmsnorm_rstd(W.h1, W.ss, W.rstd, W.junk)
                STT(W.xn2[:], W.h1[:], W.rstd[:], gx[:], ALU.mult, ALU.mult, [W.h1.b, W.rstd.b, gx.b], [W.xn2.b])
                transpose8(W.xn2, W.xn2T, TB, evac="dve")
                yield
                for h4 in range(4):
                    mm_acc(PS[C][:, h4 * 128:(h4 + 1) * 128], [(w_xq[:, c, h4 * 128:(h4 + 1) * 128], W.xn2T[:, c, :]) for c in range(8)],
                           [w_xq.b, W.xn2T.b], [PS[C].b])
                ACTF(W.q2T[:].rearrange("p h t -> p (h t)"), PS[C][:], AF.Copy, [PS[C].b], [W.q2T.b], scale=SCALE)
                yield
                for h4 in range(4):
                    for mh in range(2):
                        idx = h4 * 2 + mh
                        bk = AB[idx // 4]
                        MM(PS[bk][:, (idx % 4) * 128:(idx % 4 + 1) * 128], memKT[:, h4, mh * 128:(mh + 1) * 128], W.q2T[:, h4, :],
                           [memKT.b, W.q2T.b], [PS[bk].b], silent=(idx % 4 != 3))
                for bb in range(2):
                    ACTF(W.P2T[:, bb * 4:(bb + 1) * 4, :].rearrange("p a t -> p (a t)"), PS[AB[bb]][:], AF.Exp, [PS[AB[bb]].b], [W.P2T.b])
                yield
                for h4 in range(4):
                    bk = AB[h4 // 2]
                    off = (h4 % 2) * 129
                    for mh in range(2):
                        MM(PS[bk][:, off:off + 129], W.P2T[:, h4 * 2 + mh, :], memV[:, mh, h4, :], [W.P2T.b, memV.b], [PS[bk].b],
                           start=(mh == 0), stop=(mh == 1), silent=(mh == 0))
                for h4 in range(4):
                    bk = AB[h4 // 2]
                    off = (h4 % 2) * 129
                    RCP(W.rc2[:, h4:h4 + 1], PS[bk][:, off + 128:off + 129], [PS[bk].b], [W.rc2.b])
                    TS(W.ox[:, h4 * 128:(h4 + 1) * 128], PS[bk][:, off:off + 128], W.rc2[:, h4:h4 + 1], None, ALU.mult, None,
                       [PS[bk].b, W.rc2.b], [W.ox.b])
                transpose8(W.ox, W.oxT, TB, n=4)
                yield
                for hf, bk in enumerate(AB):
                    mm_acc(PS[bk][:], [(W.oxT[:, c, :], w_xo[:, c, hf * 512:(hf + 1) * 512]) for c in range(4)],
                           [W.oxT.b, w_xo.b], [PS[bk].b])
                for hf, bk in enumerate(AB):
                    cs = slice(hf * 512, (hf + 1) * 512)
                    TT(W.h2[:, cs], PS[bk][:], W.h1[:, cs], ALU.add, [PS[bk].b, W.h1.b], [W.h2.b])
                DMA("pool", h2_d[rows, :], W.h2[:], [W.h2.b], [], "dps")
                yield
                rmsnorm_rstd(W.h2, W.ss, W.rstd, W.junk)
                STT(W.xn3f[:], W.h2[:], W.rstd[:], gffn[:], ALU.mult, ALU.mult, [W.h2.b, W.rstd.b, gffn.b], [W.xn3f.b])
                ACTC(xn3b[p_][:], W.xn3f[:], [W.xn3f.b], [xn3b[p_].b])
                for c in range(8):
                    bk = AB[c // 4]
                    TR(PS[bk][:, (c % 4) * 128:(c % 4 + 1) * 128], W.xn3f[:, c * 128:(c + 1) * 128], identf[:], [W.xn3f.b, identf.b], [PS[bk].b],
                       silent=(c % 4 != 3))
                CP(W.xn3T[:, 0:4, :].rearrange("p c t -> p (c t)"), PS[A][:], [PS[A].b], [W.xn3T.b])
                ACTC(W.xn3T[:, 4:8, :].rearrange("p c t -> p (c t)"), PS[B_][:], [PS[B_].b], [W.xn3T.b])
                yield
                mm_acc(PS[C][:, 0:72], [(W.xn3T[:, c, :], w_r[:, c, :]) for c in range(8)], [W.xn3T.b, w_r.b], [PS[C].b])
                TT(W.lg[:], PS[C][:, 0:72], b_r[:], ALU.add, [PS[C].b, b_r.b], [W.lg.b])
                RED(W.gmax[:], W.lg[:, 0:8], ALU.max, [W.lg.b], [W.gmax.b])
                TS(W.ohg[:], W.lg[:, 0:8], W.gmax[:], None, ALU.is_equal, None, [W.lg.b, W.gmax.b], [W.ohg.b])
                TS(W.ngmax[:], W.gmax[:], -1.0, None, ALU.mult, None, [W.gmax.b], [W.ngmax.b])
                ACTF(W.eg[:], W.lg[:, 0:8], AF.Exp, [W.lg.b, W.ngmax.b], [W.eg.b, W.sumg.b], bias=W.ngmax[:], accum_out=W.sumg[:])
                RCP(W.pg[:], W.sumg[:], [W.sumg.b], [W.pg.b])
                TS(W.pen[:], W.ohg[:], 1e30, -1e30, ALU.mult, ALU.add, [W.ohg.b], [W.pen.b])
                TT(W.em[:].rearrange("p (g e) -> p g e", g=8), W.lg[:, 8:72].rearrange("p (g e) -> p g e", g=8),
                   W.pen[:, :].unsqueeze(2).to_broadcast([128, 8, 8]), ALU.add, [W.lg.b, W.pen.b], [W.em.b])
                S.op("dve", lambda e, W=W: e.max(out=W.tp8[:], in_=W.em[:]), r=[W.em.b], w=[W.tp8.b])
                TS(W.sel1[:], W.em[:], W.tp8[:, 0:1], None, ALU.is_equal, None, [W.em.b, W.tp8.b], [W.sel1.b])
                TS(W.sel2[:], W.em[:], W.tp8[:, 1:2], None, ALU.is_equal, None, [W.em.b, W.tp8.b], [W.sel2.b])
                TT(W.dd[:], W.tp8[:, 1:2], W.tp8[:, 0:1], ALU.subtract, [W.tp8.b], [W.dd.b])
                ACTF(W.ee[:], W.dd[:], AF.Exp, [W.dd.b], [W.ee.b])
                TS(W.w1[:], W.ee[:], 1.0, None, ALU.add, None, [W.ee.b], [W.w1.b])
                RCP(W.w1[:], W.w1[:], [W.w1.b], [W.w1.b])
                TS(W.w2[:], W.w1[:], -1.0, 1.0, ALU.mult, ALU.add, [W.w1.b], [W.w2.b])
                TT(wts[:, i, 0:1], W.w1[:], W.pg[:], ALU.mult, [W.w1.b, W.pg.b], [wts.b])
                TT(wts[:, i, 1:2], W.w2[:], W.pg[:], ALU.mult, [W.w2.b, W.pg.b], [wts.b])
                TT(W.selb[:], W.sel1[:], W.sel2[:], ALU.add, [W.sel1.b, W.sel2.b], [W.selb.b])
                MM(PS[C][:, 128:192], ltri[:], W.selb[:], [ltri.b, W.selb.b], [PS[C].b])
                MM(PS[C][:, 256:320], onesb[:], W.selb[:], [onesb.b, W.selb.b], [PS[C].b])
                TT(W.posf[:], PS[C][:, 128:192], cum[:], ALU.add, [PS[C].b, cum.b], [W.posf.b])
                TT(cum[:], cum[:], PS[C][:, 256:320], ALU.add, [PS[C].b, cum.b], [cum.b])
                TS(W.over[:], W.posf[:], float(CAP), 1e6, ALU.is_ge, ALU.mult, [W.posf.b], [W.over.b])
                TT(W.posf[:], W.posf[:], W.over[:], ALU.add, [W.posf.b, W.over.b], [W.posf.b])
                TT(W.posf[:], W.posf[:], ebase[:], ALU.add, [W.posf.b, ebase.b], [W.posf.b])
                TT(W.prod[:], W.posf[:], W.sel1[:], ALU.mult, [W.posf.b, W.sel1.b], [W.prod.b])
                RED(W.slf[:, 0:1], W.prod[:], ALU.add, [W.prod.b], [W.slf.b])
                TT(W.prod[:], W.posf[:], W.sel2[:], ALU.mult, [W.posf.b, W.sel2.b], [W.prod.b])
                RED(W.slf[:, 1:2], W.prod[:], ALU.add, [W.prod.b], [W.slf.b])
                CP(slots[:, 2 * i:2 * i + 2], W.slf[:], [W.slf.b], [slots.b])
                for kk in range(2):
                    S.op("pool", lambda e, i=i, kk=kk, p_=p_: e.indirect_dma_start(
                        out=xbuf_d, out_offset=bass.IndirectOffsetOnAxis(ap=slots[:, 2 * i + kk:2 * i + kk + 1], axis=0),
                        in_=xn3b[p_][:, :], in_offset=None, bounds_check=S.regs["bnd"], oob_is_err=False),
                        r=[xn3b[p_].b, slots.b], w=[xbufb], sem="dg", inc=16)
                yield

            SKEW = 5
            active = []
            nxt = 0
            while nxt < NT or active:
                if nxt < NT and len(active) < 2 and (not active or active[-1][1] >= SKEW):
                    active.append([tile3(nxt), 0])
                    nxt += 1
                for a_ in list(active):
                    try:
                        next(a_[0])
                        a_[1] += 1
                    except StopIteration:
                        active.remove(a_)
            emit_casts(10 ** 6)
            if debug:
                DMA("sp", dbg_rt[:, :, 0:2], wts[:], [wts.b], [], "dst")
            S.barrier()

        with ExitStack() as p5:
            NWB = 3
            wg = [sb(p5, "wg%d" % i, [128, 8, 512], BF16) for i in range(NWB)]
            wu = [sb(p5, "wu%d" % i, [128, 8, 512], BF16) for i in range(NWB)]
            wd = [sb(p5, "wd%d" % i, [128, 4, 1024], BF16) for i in range(NWB)]
            xe = [sb(p5, "xe%d" % i, [128, D], BF16) for i in range(4)]
            xeT = [sb(p5, "xeT%d" % i, [128, 8, 256], BF16) for i in range(2)]
            sgl = sb(p5, "sgl", [128, 1024], F32)
            hdn = [sb(p5, "hdn%d" % i, [128, 4, 256], BF16) for i in range(2)]
            yb = [sb(p5, "yb%d" % i, [128, D], BF16) for i in range(2)]
            def load_x(ex):
                for blk in range(2):
                    xb = xe[(2 * ex + blk) % 4]
                    r0 = ex * CAP + blk * 128
                    DMA("sp", xb[:], xbuf_d[r0:r0 + 128, :], [], [xb.b], "dx")

            def load_w(ex_):
                kw = ex_ % NWB
                DMA("pool", wg[kw][:].rearrange("p c n -> p (c n)"), wgb_d[ex_], [], [wg[kw].b], "dw")
                DMA("act", wu[kw][:].rearrange("p c n -> p (c n)"), wub_d[ex_], [], [wu[kw].b], "dq")
                DMA("sp", wd[kw][:].rearrange("p c n -> p (c n)"), wdb_d[ex_], [], [wd[kw].b], "dc")

            load_w(0)
            load_w(1)
            for ex in range(NEXP):
                k = ex % NWB
                k2 = ex % 2
                if ex + 2 < NEXP:
                    load_w(ex + 2)
                if ex == 0:
                    load_x(0)
                if ex + 1 < NEXP:
                    load_x(ex + 1)
                for blk in range(2):
                    xb = xe[(2 * ex + blk) % 4]
                    pv = psb(7)
                    for c in range(8):
                        TR(pv[:, c * 128:(c + 1) * 128], xb[:, c * 128:(c + 1) * 128], ident[:], [xb.b, ident.b], [PS[7].b], silent=(c < 7))
                    if blk == 0:
                        ACTC(xeT[k2][:, :, 0:128], pv[:, :].rearrange("p (c t) -> p c t", c=8), [PS[7].b], [xeT[k2].b])
                    else:
                        CP(xeT[k2][:, :, 128:256], pv[:, :].rearrange("p (c t) -> p c t", c=8), [PS[7].b], [xeT[k2].b])
                for hc in range(4):
                    cs = slice((hc % 2) * 256, (hc % 2 + 1) * 256)
                    mm_acc(PS[hc // 2][:, cs], [(wg[k][:, c, hc * 128:(hc + 1) * 128], xeT[k2][:, c, :]) for c in range(8)],
                           [wg[k].b, xeT[k2].b], [PS[hc // 2].b])
                for hc in range(4):
                    cs = slice((hc % 2) * 256, (hc % 2 + 1) * 256)
                    mm_acc(PS[2 + hc // 2][:, cs], [(wu[k][:, c, hc * 128:(hc + 1) * 128], xeT[k2][:, c, :]) for c in range(8)],
                           [wu[k].b, xeT[k2].b], [PS[2 + hc // 2].b])
                for b2 in range(2):
                    cs = slice(b2 * 512, (b2 + 1) * 512)
                    ACTF(sgl[:, cs], PS[b2][:], AF.Silu, [PS[b2].b], [sgl.b])
                    TT(hdn[k2][:, 2 * b2:2 * b2 + 2, :].rearrange("p a t -> p (a t)"), sgl[:, cs], PS[2 + b2][:], ALU.mult,
                       [sgl.b, PS[2 + b2].b], [hdn[k2].b])
                for blk in range(2):
                    ybk = yb[blk]
                    for hf in range(2):
                        mm_acc(PS[4 + hf][:], [(hdn[k2][:, hc, blk * 128:(blk + 1) * 128], wd[k][:, hc, hf * 512:(hf + 1) * 512]) for hc in range(4)],
                               [hdn[k2].b, wd[k].b], [PS[4 + hf].b])
                    ACTC(ybk[:, 0:512], PS[4][:], [PS[4].b], [ybk.b])
                    CP(ybk[:, 512:1024], PS[5][:], [PS[5].b], [ybk.b])
                    r0 = ex * CAP + blk * 128
                    DMA("sp", ybuf_d[r0:r0 + 128, :], ybk[:], [ybk.b], [], "dst")
            S.barrier()

        with ExitStack() as p6:
            h2t = [sb(p6, "h2t%d" % i, [128, D], F32) for i in range(2)]
            h3 = sb(p6, "h3", [128, D], F32)
            junk5 = sb(p6, "junk5", [128, D], F32)
            ss5 = sb(p6, "ss5", [128, 1], F32)
            rstd5 = sb(p6, "rstd5", [128, 1], F32)
            ot = [sb(p6, "ot%d" % i, [128, D], F32) for i in range(2)]
            for i in range(NT):
                k = i % 2
                rows = slice(i * 128, (i + 1) * 128)
                DMA("sp", h2t[k][:], h2_d[rows, :], [], [h2t[k].b], "dx")
                MSET(y1[k][:], 0.0, [y1[k].b])
                MSET(y2[k][:], 0.0, [y2[k].b])
                for kk, yy in ((0, y1[k]), (1, y2[k])):
                    S.op("pool", lambda e, i=i, kk=kk, yy=yy: e.indirect_dma_start(
                        out=yy[:, :], out_offset=None, in_=ybuf_d,
                        in_offset=bass.IndirectOffsetOnAxis(ap=slots[:, 2 * i + kk:2 * i + kk + 1], axis=0),
                        bounds_check=S.regs["bnd"], oob_is_err=False),
                        r=[slots.b], w=[yy.b], sem="dg", inc=16)
                STT(h3[:], y1[k][:], wts[:, i, 0:1], h2t[k][:], ALU.mult, ALU.add, [y1[k].b, wts.b, h2t[k].b], [h3.b])
                STT(h3[:], y2[k][:], wts[:, i, 1:2], h3[:], ALU.mult, ALU.add, [y2[k].b, wts.b, h3.b], [h3.b])
                rmsnorm_rstd(h3, ss5, rstd5, junk5)
                STT(ot[k][:], h3[:], rstd5[:], gfin[:], ALU.mult, ALU.mult, [h3.b, rstd5.b, gfin.b], [ot[k].b])
                DMA("act", out_d[rows, :], ot[k][:], [ot[k].b], [], "dst")

        block = es.enter_context(nc.Block())
        S.flush(block)
    return nc


_NC_CACHE = {}


def kernel(**inputs):
    f32 = lambda a: np.ascontiguousarray(np.asarray(a), dtype=np.float32)
    x = f32(inputs["x"])
    mem = f32(inputs["mem"])
    B = x.shape[0]
    shared = {}
    for k, v in inputs.items():
        if k in ("x", "mem"):
            continue
        a = f32(v)
        if k in ("rel_bias", "norm_final"):
            shared[k] = a
        else:
            shared[k] = np.ascontiguousarray(a[0])
    shared.update({k: f32(v) for k, v in host_consts().items()})
    if "nc" not in _NC_CACHE:
        _NC_CACHE["nc"] = build(debug=False)
    nc = _NC_CACHE["nc"]
    in_maps = []
    for b in range(B):
        m = dict(shared)
        m["x"] = np.ascontiguousarray(x[b])
        m["mem"] = np.ascontiguousarray(mem[b])
        in_maps.append(m)
    res = run_bass_kernel_spmd(nc, in_maps, core_ids=list(range(B)))
    out = np.stack([np.asarray(r["out"], dtype=np.float32) for r in res.results], axis=0)
    return out
```

```python
import math
from contextlib import ExitStack

import numpy as np
import concourse.bass as bass
import concourse.mybir as mybir
from concourse.bass_utils import run_bass_kernel_spmd

F32 = mybir.dt.float32
BF16 = mybir.dt.bfloat16
I32 = mybir.dt.int32
AF = mybir.ActivationFunctionType
ALU = mybir.AluOpType
AX = mybir.AxisListType

S_LEN = 4096
D = 1024
NT = S_LEN // 128
NEXP = 64
CAP = 256
NSLOT = NEXP * CAP
NEGM = -30000.0


class Buf:
    __slots__ = ("lw", "rd")

    def __init__(self):
        self.lw = None
        self.rd = []


class Sched:
    ENG = ("pe", "act", "dve", "pool", "sp")
    SAME_ENGINE_SYNC = True

    def __init__(self, nc, semh):
        self.nc = nc
        self.semh = semh
        self.cnt = {k: 0 for k in semh}
        self.streams = {e: [] for e in self.ENG}
        self.waited = {e: {} for e in self.ENG}
        self.pools = {}
        self.regs = {}
        self.rr = {}
        for k in semh:
            if "_" in k:
                base = k.rsplit("_", 1)[0]
                self.pools.setdefault(base, []).append(k)
                self.rr[base] = 0

    def op(self, eng, fn, r=(), w=(), sem=None, inc=1, silent=False):
        dma = sem is not None
        if dma:
            pool = self.pools[sem]
            sem = pool[self.rr[sem] % len(pool)]
            self.rr[sem.rsplit("_", 1)[0]] += 1
        else:
            sem = eng
        waits = {}

        def need(ev):
            if ev is None:
                return
            s, v = ev
            if v > waits.get(s, 0):
                waits[s] = v

        for b in r:
            need(b.lw)
        for b in w:
            need(b.lw)
            for ev in b.rd:
                need(ev)
        if dma and self.cnt[sem] > 0:
            need((sem, self.cnt[sem]))
        myv = self.cnt[sem] + inc
        if not silent:
            self.cnt[sem] = myv
        ev = (sem, myv)
        for b in r:
            b.rd.append(ev)
        for b in w:
            b.lw = ev
            b.rd = []
        wd = self.waited[eng]
        wl = []
        for s, v in waits.items():
            if s == eng and (eng == "pe" or not self.SAME_ENGINE_SYNC):
                continue
            if s == sem and v >= myv:
                continue
            if wd.get(s, 0) >= v:
                continue
            wd[s] = v
            wl.append((s, v))
        self.streams[eng].append((wl, fn, sem, inc, silent))

    def barrier(self):
        snap = dict(self.cnt)
        for eng in self.ENG:
            wd = self.waited[eng]
            wl = []
            for s, v in snap.items():
                if v > 0 and wd.get(s, 0) < v:
                    wd[s] = v
                    wl.append((s, v))
            if wl:
                self.streams[eng].append((wl, None, None, 0, True))

    def flush(self, block):
        decos = {"pe": block.tensor, "act": block.scalar, "dve": block.vector,
                 "pool": block.gpsimd, "sp": block.sync}
        for name in self.ENG:
            ops = self.streams[name]
            final = dict(self.cnt) if name == "sp" else None

            def body(e, ops=ops, final=final, name=name):
                if name == "pool":
                    breg = e.alloc_register("bnd")
                    e.reg_mov(breg, NSLOT - 1)
                    self.regs["bnd"] = breg
                for wl, fn, sem, inc, silent in ops:
                    attach = (fn is not None and bool(wl) and name in ("act", "dve", "pool") and sem == name)
                    for s, v in (wl[:-1] if attach else wl):
                        e.wait_ge(self.semh[s], v)
                    if fn is None:
                        continue
                    ins = fn(e)
                    if attach:
                        ins._wait_ge(self.semh[wl[-1][0]], wl[-1][1])
                    if not silent:
                        ins.then_inc(self.semh[sem], inc)
                if final is not None:
                    for s, v in final.items():
                        if v > 0:
                            e.wait_ge(self.semh[s], v)

            decos[name](body)


class T:
    __slots__ = ("t", "b")

    def __init__(self, t):
        self.t = t
        self.b = Buf()

    def __getitem__(self, k):
        return self.t[k]


DMA_POOLS = {"dx": 4, "dw": 8, "dst": 8, "dps": 8, "dq": 6, "dg": 4, "dz": 4, "dc": 8, "dcw": 8}
SEM_NAMES = ["pe", "act", "dve", "pool", "sp"] + ["%s_%d" % (k, i) for k, n in DMA_POOLS.items() for i in range(n)]


def rel_bucket_np(dist):
    n = np.maximum(dist, 0)
    max_exact = 16
    nf = np.maximum(n, 1).astype(np.float32)
    large = max_exact + (np.log(nf / max_exact) / math.log(128 / max_exact) * (32 - max_exact)).astype(np.int32)
    large = np.minimum(large, 31)
    return np.where(n < max_exact, n, large)


def host_consts():
    c = {}
    k = np.arange(128)[:, None]
    q = np.arange(128)[None, :]
    oh = np.zeros((32, 2, 128, 128), np.float32)
    bd = rel_bucket_np(q - k)
    bp = rel_bucket_np(q + 128 - k)
    for r in range(32):
        oh[r, 0] = ((bd == r) & (q >= k))
        oh[r, 1] = (bp == r)
    c["c_oh"] = oh.reshape(32, 2 * 128 * 128)
    c["c_negT"] = np.where(q < k, NEGM, 0.0).astype(np.float32)
    c["c_tri"] = (k <= q).astype(np.float32)
    c["c_ltri"] = (k < q).astype(np.float32)
    pm = np.zeros((16, 16), np.float32)
    ps = np.zeros((16, 16), np.float32)
    for cc in range(16):
        pm[cc, cc:] = -1e30
        pm[cc, cc] = 1e30
        ps[cc, :cc] = 1.0
    c["c_pm"] = np.broadcast_to(pm.reshape(1, 256), (128, 256)).copy()
    c["c_psel"] = np.broadcast_to(ps.reshape(1, 256), (128, 256)).copy()
    es = np.zeros((16, 16, 128), np.float32)
    for n in range(16):
        es[n, n, :] = 1.0
    c["c_esel"] = es.reshape(16, 16 * 128)
    c["c_ebase"] = np.broadcast_to((np.arange(64) * CAP).astype(np.float32)[None, :], (128, 64)).copy()
    return c


def build(debug=False):
    nc = bass.Bass("TRN2", target_bir_lowering=False)

    def din(name, shape, dt=F32):
        return nc.dram_tensor(name, list(shape), dt, kind="ExternalInput").ap()

    def dscr(name, shape, dt):
        return nc.dram_tensor(name, list(shape), dt, kind=("ExternalOutput" if debug else "Internal")).ap()

    x_d = din("x", [S_LEN, D])
    mem_d = din("mem", [256, D])
    norm_mix_d = din("norm_mix", [D])
    w_in_d = din("w_in", [D, 5120])
    w_gate_d = din("w_gate", [D, 2048])
    b_gate_d = din("b_gate", [2048])
    sgu_norm_d = din("sgu_norm", [D])
    w_sp_d = din("w_spatial", [8, 128, 128])
    b_sp_d = din("b_spatial", [8, 128])
    w_a_d = din("w_branch_a", [D, D])
    w_b_d = din("w_branch_b", [D, D])
    relb_d = din("rel_bias", [32, 8])
    w_o_d = din("w_o", [D, D])
    norm_x_d = din("norm_x", [D])
    norm_mem_d = din("norm_mem", [D])
    w_xq_d = din("w_xq", [D, 512])
    w_xkv_d = din("w_xkv", [D, 1024])
    w_xo_d = din("w_xo", [512, D])
    norm_ffn_d = din("norm_ffn", [D])
    w_rg_d = din("w_router_group", [D, 8])
    b_rg_d = din("b_router_group", [8])
    w_re_d = din("w_router_expert", [D, 64])
    b_re_d = din("b_router_expert", [64])
    w_eg_d = din("w_e_gate", [NEXP, D, 512])
    w_eu_d = din("w_e_up", [NEXP, D, 512])
    w_ed_d = din("w_e_down", [NEXP, 512, D])
    norm_fin_d = din("norm_final", [D])
    c_oh_d = din("c_oh", [32, 32768])
    c_negT_d = din("c_negT", [128, 128])
    c_tri_d = din("c_tri", [128, 128])
    c_ltri_d = din("c_ltri", [128, 128])
    c_pm_d = din("c_pm", [128, 256])
    c_psel_d = din("c_psel", [128, 256])
    c_esel_d = din("c_esel", [16, 2048])
    c_ebase_d = din("c_ebase", [128, 64])

    out_d = nc.dram_tensor("out", [S_LEN, D], F32, kind="ExternalOutput").ap()

    xnT_d = dscr("xnT_s", [8, 128, S_LEN], BF16)
    ga_d = dscr("ga_s", [S_LEN, D], BF16)
    g1_d = dscr("g1_s", [S_LEN, D], BF16)
    qT_d = dscr("qT_s", [8, 128, S_LEN], BF16)
    kT_d = dscr("kT_s", [8, 128, S_LEN], BF16)
    v_d = dscr("v_s", [8, 128, NT, 129], BF16)
    bt_d = dscr("bt_s", [8, 2, 128, 128], F32)
    h2_d = dscr("h2_s", [S_LEN, D], F32)
    xbuf_d = dscr("xbuf_s", [NSLOT, D], BF16)
    wgb_d = nc.dram_tensor("wgb_s", [NEXP, 128, 8 * 512], BF16, kind="Internal").ap()
    wub_d = nc.dram_tensor("wub_s", [NEXP, 128, 8 * 512], BF16, kind="Internal").ap()
    wdb_d = nc.dram_tensor("wdb_s", [NEXP, 128, 4 * 1024], BF16, kind="Internal").ap()
    ybuf_d = dscr("ybuf_s", [NSLOT, D], BF16)
    att_d = dscr("att_s", [8, 128, S_LEN], BF16)
    dbg_rt = dscr("rt_s", [128, NT, 4], F32) if debug else None

    with ExitStack() as es:
        semh = {n: es.enter_context(nc.semaphore(n)) for n in SEM_NAMES}
        S = Sched(nc, semh)

        uid = [0]

        def sb(stack, name, shape, dt):
            uid[0] += 1
            return T(stack.enter_context(nc.sbuf_tensor("s%d_%s" % (uid[0], name), list(shape), dt)))

        def DMA(eng, out, in_, r, w, sem):
            S.op(eng, lambda e: e.dma_start(out=out, in_=in_), r=r, w=w, sem=sem, inc=16)

        def mm_acc(out_ap, pairs, r, w):
            n = len(pairs)
            for i, (l, rh) in enumerate(pairs):
                S.op("pe", lambda e, l=l, rh=rh, i=i: e.matmul(out_ap, lhsT=l, rhs=rh, start=(i == 0), stop=(i == n - 1)),
                     r=r, w=w, silent=(i < n - 1))

        cast_list = []
        for ex_ in range(NEXP):
            cast_list += [(wgb_d[ex_].rearrange("p (c n) -> p c n", c=8), w_eg_d[ex_].rearrange("(c p) n -> p c n", p=128)),
                          (wub_d[ex_].rearrange("p (c n) -> p c n", c=8), w_eu_d[ex_].rearrange("(c p) n -> p c n", p=128)),
                          (wdb_d[ex_].rearrange("p (c n) -> p c n", c=4), w_ed_d[ex_].rearrange("(c p) n -> p c n", p=128))]
        cast_pos = [0]

        def emit_casts(k):
            for _ in range(k):
                if cast_pos[0] < len(cast_list):
                    o_, i_ = cast_list[cast_pos[0]]
                    cast_pos[0] += 1
                    DMA("pool", o_, i_, [], [], "dcw")

        def ACTF(out, in_, func, r, w, **kw):
            S.op("act", lambda e: e.activation(out=out, in_=in_, func=func, **kw), r=r, w=w)

        def ACTC(out, in_, r, w):
            S.op("act", lambda e: e.copy(out=out, in_=in_), r=r, w=w)

        def TT(out, in0, in1, op, r, w, eng="dve"):
            S.op(eng, lambda e: e.tensor_tensor(out=out, in0=in0, in1=in1, op=op), r=r, w=w)

        def TS(out, in0, s1, s2, op0, op1, r, w, eng="dve"):
            if op1 is None:
                S.op(eng, lambda e: e.tensor_scalar(out=out, in0=in0, scalar1=s1, scalar2=None, op0=op0), r=r, w=w)
            else:
                S.op(eng, lambda e: e.tensor_scalar(out=out, in0=in0, scalar1=s1, scalar2=s2, op0=op0, op1=op1), r=r, w=w)

        def STT(out, in0, scalar, in1, op0, op1, r, w, eng="dve"):
            S.op(eng, lambda e: e.scalar_tensor_tensor(out=out, in0=in0, scalar=scalar, in1=in1, op0=op0, op1=op1), r=r, w=w)

        def CP(out, in_, r, w, eng="dve"):
            S.op(eng, lambda e: e.tensor_copy(out=out, in_=in_), r=r, w=w)

        def RED(out, in_, op, r, w):
            S.op("dve", lambda e: e.tensor_reduce(out=out, in_=in_, axis=AX.X, op=op), r=r, w=w)

        def RCP(out, in_, r, w):
            S.op("dve", lambda e: e.reciprocal(out=out, in_=in_), r=r, w=w)

        def MM(out, lhsT, rhs, r, w, start=True, stop=True, silent=False, skip=False):
            if skip:
                S.op("pe", lambda e: e.matmul(out, lhsT=lhsT, rhs=rhs, start=start, stop=stop, skip_group_check=True),
                     r=r, w=w, silent=silent)
            else:
                S.op("pe", lambda e: e.matmul(out, lhsT=lhsT, rhs=rhs, start=start, stop=stop), r=r, w=w, silent=silent)

        def TR(out, in_, idn, r, w, silent=False):
            S.op("pe", lambda e: e.transpose(out=out, in_=in_, identity=idn), r=r, w=w, silent=silent)

        def MSET(t_ap, val, w, eng="pool"):
            S.op(eng, lambda e: e.memset(t_ap, val), w=w)

        PS = [T(es.enter_context(nc.psum_tensor("ps%d" % i, [128, 512], F32))) for i in range(8)]

        def psb(i):
            return PS[i].t[:].bitcast(BF16)

        identf = sb(es, "identf", [128, 128], F32)
        ident = sb(es, "ident", [128, 128], BF16)
        onesb = sb(es, "onesb", [128, 128], BF16)
        slots = sb(es, "slots", [128, NT * 2], I32)
        wts = sb(es, "wts", [128, NT, 2], F32)
        gfin = sb(es, "gfin", [128, D], F32)
        xn3b = [sb(es, "xn3b%d" % i, [128, D], BF16) for i in range(2)]
        y1 = [sb(es, "y1_%d" % i, [128, D], BF16) for i in range(2)]
        y2 = [sb(es, "y2_%d" % i, [128, D], BF16) for i in range(2)]

        S.op("pool", lambda e: e.memset(identf[:], 0.0), w=[identf.b])
        S.op("pool", lambda e: e.affine_select(out=identf[:], in_=identf[:], pattern=[[-1, 128]], compare_op=ALU.not_equal,
                                                fill=1.0, base=0, channel_multiplier=1), r=[identf.b], w=[identf.b])
        S.op("dve", lambda e: e.tensor_copy(out=ident[:], in_=identf[:]), r=[identf.b], w=[ident.b])
        S.op("dve", lambda e: e.memset(onesb[:], 1.0), w=[onesb.b])
        DMA("sp", gfin[:], norm_fin_d.partition_broadcast(128), [], [gfin.b], "dc")

        def rmsnorm_rstd(src, ss, rstd, junk):
            S.op("act", lambda e: e.activation(out=junk[:], in_=src[:], func=AF.Square, accum_out=ss[:]),
                 r=[src.b], w=[junk.b, ss.b])
            S.op("act", lambda e: e.activation(out=rstd[:], in_=ss[:], func=AF.Sqrt, bias=1e-6, scale=1.0 / D),
                 r=[ss.b], w=[rstd.b])
            S.op("dve", lambda e: e.reciprocal(out=rstd[:], in_=rstd[:]), r=[rstd.b], w=[rstd.b])

        def transpose8(src_bf, dstT, bank, n=8, evac="act"):
            pv = psb(bank)
            for c in range(n):
                S.op("pe", lambda e, c=c: e.transpose(out=pv[:, c * 128:(c + 1) * 128], in_=src_bf[:, c * 128:(c + 1) * 128],
                                                      identity=ident[:]),
                     r=[src_bf.b, ident.b], w=[PS[bank].b], silent=(c < n - 1))
            if evac == "act":
                S.op("act", lambda e: e.copy(out=dstT[:].rearrange("p c t -> p (c t)"), in_=pv[:, 0:n * 128]),
                     r=[PS[bank].b], w=[dstT.b])
            else:
                S.op("dve", lambda e: e.tensor_copy(out=dstT[:].rearrange("p c t -> p (c t)"), in_=pv[:, 0:n * 128]),
                     r=[PS[bank].b], w=[dstT.b])

        with ExitStack() as p1:
            w_uv = sb(p1, "w_uv", [128, 8, 2048], BF16)
            w_g = sb(p1, "w_g", [128, 8, 2048], BF16)
            w_a = sb(p1, "w_a", [128, 8, 1024], BF16)
            wspT = sb(p1, "wspT", [128, 8, 128], BF16)
            wsp_f = sb(p1, "wsp_f", [128, 8, 128], F32)
            tri = sb(p1, "tri", [128, 128], F32)
            bspT = sb(p1, "bspT", [128, 8], F32)
            bsp_f = sb(p1, "bsp_f", [8, 128], F32)
            gmix = sb(p1, "gmix", [128, D], F32)
            gsgu = sb(p1, "gsgu", [128, D], F32)
            bgate = sb(p1, "bgate", [128, 2048], F32)
            wuv_b = [Buf() for _ in range(4)]
            wg_b = [Buf() for _ in range(4)]
            wa_b = [Buf() for _ in range(2)]
            for nb in range(4):
                cs_ = slice(nb * 512, (nb + 1) * 512)
                DMA("pool", w_uv[:, :, cs_], w_in_d[:, cs_].rearrange("(c p) n -> p c n", p=128), [], [wuv_b[nb]], "dw")
            for nb in range(4):
                cs_ = slice(nb * 512, (nb + 1) * 512)
                DMA("pool", w_g[:, :, cs_], w_gate_d[:, cs_].rearrange("(c p) n -> p c n", p=128), [], [wg_b[nb]], "dw")
            for hf in range(2):
                cs_ = slice(hf * 512, (hf + 1) * 512)
                DMA("pool", w_a[:, :, cs_], w_a_d[:, cs_].rearrange("(c p) n -> p c n", p=128), [], [wa_b[hf]], "dw")
            DMA("sp", wsp_f[:], w_sp_d.rearrange("g t s -> t g s"), [], [wsp_f.b], "dc")
            DMA("sp", tri[:], c_tri_d, [], [tri.b], "dc")
            DMA("sp", bsp_f[:], b_sp_d, [], [bsp_f.b], "dc")
            DMA("sp", gmix[:], norm_mix_d.partition_broadcast(128), [], [gmix.b], "dc")
            DMA("sp", gsgu[:], sgu_norm_d.partition_broadcast(128), [], [gsgu.b], "dc")
            DMA("sp", bgate[:], b_gate_d.partition_broadcast(128), [], [bgate.b], "dc")
            for g in range(8):
                S.op("pe", lambda e, g=g: e.transpose(out=PS[0][:, g * 128:(g + 1) * 128] if g < 4 else PS[1][:, (g - 4) * 128:(g - 3) * 128],
                                                      in_=wsp_f[:, g, :], identity=identf[:]),
                     r=[wsp_f.b, identf.b], w=[PS[0].b if g < 4 else PS[1].b])
            for g in range(8):
                src = PS[0] if g < 4 else PS[1]
                gg = g % 4
                S.op("dve", lambda e, g=g, src=src, gg=gg: e.tensor_tensor(out=wspT[:, g, :], in0=src[:, gg * 128:(gg + 1) * 128],
                                                                          in1=tri[:], op=ALU.mult),
                     r=[src.b, tri.b], w=[wspT.b])
            S.op("pe", lambda e: e.transpose(out=PS[2][:, 0:8], in_=bsp_f[:, :], identity=identf[0:8, 0:8]),
                 r=[bsp_f.b, identf.b], w=[PS[2].b])
            S.op("dve", lambda e: e.tensor_copy(out=bspT[:], in_=PS[2][:, 0:8]), r=[PS[2].b], w=[bspT.b])

            NB = 2
            xt = [sb(p1, "xt%d" % i, [128, D], F32) for i in range(NB)]
            junk = sb(p1, "junk1", [128, D], F32)
            ss = [sb(p1, "ss%d" % i, [128, 1], F32) for i in range(NB)]
            rstd = [sb(p1, "rstd%d" % i, [128, 1], F32) for i in range(NB)]
            xn = [sb(p1, "xn%d" % i, [128, D], BF16) for i in range(NB)]
            xnT = [sb(p1, "xnT%d" % i, [128, 8, 128], BF16) for i in range(NB)]
            sq = sb(p1, "sq", [128, 2048], F32)
            inner = sb(p1, "inner", [128, 2048], F32)
            sg_ = sb(p1, "sgm", [128, 2048], F32)
            uv = sb(p1, "uv", [128, 2048], F32)
            ssv = sb(p1, "ssv", [128, 1], F32)
            rstdv = sb(p1, "rstdv", [128, 1], F32)
            vn = sb(p1, "vn", [128, D], BF16)
            sgb = sb(p1, "sgb", [128, D], BF16)
            sgT = sb(p1, "sgT", [128, 8, 128], BF16)
            gpre = sb(p1, "gpre", [128, 2048], F32)
            gts = sb(p1, "gts", [128, 2048], F32)
            ga_t = [sb(p1, "ga_t%d" % i, [128, D], BF16) for i in range(NB)]
            g1_t = [sb(p1, "g1_t%d" % i, [128, D], BF16) for i in range(NB)]

            junkh = sb(p1, "junkh", [128, D], F32)

            def head1(i):
                k = i % NB
                rows = slice(i * 128, (i + 1) * 128)
                DMA("sp", xt[k][:], x_d[rows, :], [], [xt[k].b], "dx")
                rmsnorm_rstd(xt[k], ss[k], rstd[k], junkh)
                STT(xn[k][:], xt[k][:], rstd[k][:], gmix[:], ALU.mult, ALU.mult, [xt[k].b, rstd[k].b, gmix.b], [xn[k].b])
                transpose8(xn[k], xnT[k], 0)
                DMA("pool", xnT_d[:, :, rows].rearrange("c p t -> p c t"), xnT[k][:], [xnT[k].b], [], "dps")

            head1(0)
            for i in range(NT):
                k = i % NB
                rows = slice(i * 128, (i + 1) * 128)
                emit_casts(2)
                for nb in range(4):
                    mm_acc(PS[nb][:], [(xnT[k][:, c, :], w_uv[:, c, nb * 512:(nb + 1) * 512]) for c in range(8)],
                           [xnT[k].b, wuv_b[nb]], [PS[nb].b])
                for nb in range(4):
                    mm_acc(PS[4 + nb][:], [(xnT[k][:, c, :], w_g[:, c, nb * 512:(nb + 1) * 512]) for c in range(8)],
                           [xnT[k].b, wg_b[nb]], [PS[4 + nb].b])
                for nb in range(4):
                    cs = slice(nb * 512, (nb + 1) * 512)
                    S.op("act", lambda e, nb=nb, cs=cs: e.activation(out=sq[:, cs], in_=PS[nb][:], func=AF.Square),
                         r=[PS[nb].b], w=[sq.b])
                    S.op("dve", lambda e, nb=nb, cs=cs: e.scalar_tensor_tensor(out=inner[:, cs], in0=sq[:, cs], scalar=0.044715,
                                                                               in1=PS[nb][:], op0=ALU.mult, op1=ALU.mult),
                         r=[sq.b, PS[nb].b], w=[inner.b])
                    S.op("dve", lambda e, nb=nb, cs=cs: e.tensor_tensor(out=inner[:, cs], in0=inner[:, cs], in1=PS[nb][:], op=ALU.add),
                         r=[inner.b, PS[nb].b], w=[inner.b])
                S.op("act", lambda e: e.activation(out=sg_[:], in_=inner[:], func=AF.Sigmoid, scale=1.5957691216),
                     r=[inner.b], w=[sg_.b])
                for nb in range(4):
                    cs = slice(nb * 512, (nb + 1) * 512)
                    S.op("dve", lambda e, nb=nb, cs=cs: e.tensor_tensor(out=uv[:, cs], in0=sg_[:, cs], in1=PS[nb][:], op=ALU.mult),
                         r=[sg_.b, PS[nb].b], w=[uv.b])
                S.op("act", lambda e: e.activation(out=junk[:], in_=uv[:, 1024:2048], func=AF.Square, accum_out=ssv[:]),
                     r=[uv.b], w=[junk.b, ssv.b])
                S.op("act", lambda e: e.activation(out=rstdv[:], in_=ssv[:], func=AF.Sqrt, bias=1e-6, scale=1.0 / D),
                     r=[ssv.b], w=[rstdv.b])
                S.op("dve", lambda e: e.reciprocal(out=rstdv[:], in_=rstdv[:]), r=[rstdv.b], w=[rstdv.b])
                S.op("dve", lambda e: e.scalar_tensor_tensor(out=vn[:], in0=uv[:, 1024:2048], scalar=rstdv[:], in1=gsgu[:],
                                                             op0=ALU.mult, op1=ALU.mult),
                     r=[uv.b, rstdv.b, gsgu.b], w=[vn.b])
                for g in range(8):
                    bk = g // 4
                    gg = g % 4
                    S.op("pe", lambda e, g=g, bk=bk, gg=gg: e.matmul(PS[bk][:, gg * 128:(gg + 1) * 128], lhsT=wspT[:, g, :],
                                                                     rhs=vn[:, g * 128:(g + 1) * 128], start=True, stop=True),
                         r=[wspT.b, vn.b], w=[PS[bk].b], silent=(gg < 3))
                for g in range(8):
                    bk = g // 4
                    gg = g % 4
                    S.op("dve", lambda e, g=g, bk=bk, gg=gg: e.scalar_tensor_tensor(
                        out=sgb[:, g * 128:(g + 1) * 128], in0=PS[bk][:, gg * 128:(gg + 1) * 128], scalar=bspT[:, g:g + 1],
                        in1=uv[:, g * 128:(g + 1) * 128], op0=ALU.add, op1=ALU.mult),
                        r=[PS[bk].b, bspT.b, uv.b], w=[sgb.b])
                transpose8(sgb, sgT, 2)
                if i + 1 < NT:
                    head1(i + 1)
                for nb in range(4):
                    cs = slice(nb * 512, (nb + 1) * 512)
                    S.op("dve", lambda e, nb=nb, cs=cs: e.tensor_tensor(out=gpre[:, cs], in0=PS[4 + nb][:], in1=bgate[:, cs], op=ALU.add),
                         r=[PS[4 + nb].b, bgate.b], w=[gpre.b])
                S.op("act", lambda e: e.activation(out=gts[:], in_=gpre[:], func=AF.Sigmoid), r=[gpre.b], w=[gts.b])
                for hf in range(2):
                    mm_acc(PS[2 + hf][:], [(sgT[:, c, :], w_a[:, c, hf * 512:(hf + 1) * 512]) for c in range(8)],
                           [sgT.b, wa_b[hf]], [PS[2 + hf].b])
                for hf in range(2):
                    cs = slice(hf * 512, (hf + 1) * 512)
                    S.op("dve", lambda e, hf=hf, cs=cs, k=k: e.tensor_tensor(out=ga_t[k][:, cs], in0=PS[2 + hf][:], in1=gts[:, cs], op=ALU.mult),
                         r=[PS[2 + hf].b, gts.b], w=[ga_t[k].b])
                S.op("act", lambda e, k=k: e.copy(out=g1_t[k][:], in_=gts[:, 1024:2048]), r=[gts.b], w=[g1_t[k].b])
                DMA("pool", ga_d[rows, :], ga_t[k][:], [ga_t[k].b], [], "dps")
                DMA("pool", g1_d[rows, :], g1_t[k][:], [g1_t[k].b], [], "dps")
            S.barrier()


        SCALE = 1.0 / math.sqrt(128.0)
        kmsum = sb(es, "kmsum", [128, 8, 16], F32)
        pA = ExitStack()
        attT = sb(pA, "attT", [128, 8, S_LEN], BF16)
        attTb = [Buf() for _ in range(NT)]
        with ExitStack() as p2:
            w_q = sb(p2, "w_q", [128, 8, 1024], BF16)
            w_k = sb(p2, "w_k", [128, 8, 1024], BF16)
            w_v = sb(p2, "w_v", [128, 8, 1024], BF16)
            wq_b = [Buf() for _ in range(4)]
            wk_b = [Buf() for _ in range(4)]
            wv_b = [Buf() for _ in range(2)]
            for ch in range(4):
                cs_ = slice(ch * 256, (ch + 1) * 256)
                DMA("pool", w_q[:, :, cs_], w_in_d[:, 2048 + ch * 256:2048 + (ch + 1) * 256].rearrange("(c p) n -> p c n", p=128),
                    [], [wq_b[ch]], "dw")
                DMA("pool", w_k[:, :, cs_], w_in_d[:, 3072 + ch * 256:3072 + (ch + 1) * 256].rearrange("(c p) n -> p c n", p=128),
                    [], [wk_b[ch]], "dw")
            for hf in range(2):
                cs_ = slice(hf * 512, (hf + 1) * 512)
                DMA("pool", w_v[:, :, cs_], w_in_d[:, 4096 + hf * 512:4096 + (hf + 1) * 512].rearrange("(c p) n -> p c n", p=128),
                    [], [wv_b[hf]], "dw")
            xg = [sb(p2, "xg%d" % i, [128, 8, 512], BF16) for i in range(2)]
            qsb = [sb(p2, "qsb%d" % i, [128, 512], BF16) for i in range(3)]
            ksb = [sb(p2, "ksb%d" % i, [128, 512], BF16) for i in range(3)]
            vaug = [sb(p2, "vaug%d" % i, [128, 8, 129], BF16) for i in range(2)]
            for i in range(2):
                S.op("pool", lambda e, i=i: e.memset(vaug[i][:, :, 128:129], 1.0), w=[vaug[i].b])
            cnt = 0
            for tg in range(8):
                k = tg % 2
                cols = slice(tg * 512, (tg + 1) * 512)
                DMA("sp", xg[k][:], xnT_d[:, :, cols].rearrange("c p t -> p c t"), [], [xg[k].b], "dx")
                for h in range(8):
                    j = cnt % 3
                    cnt += 1
                    bq = (2 * h) % 4
                    bk = (2 * h + 1) % 4
                    hs = slice(h * 128, (h + 1) * 128)
                    mm_acc(PS[bq][:], [(w_q[:, c, hs], xg[k][:, c, :]) for c in range(8)], [wq_b[h // 2], xg[k].b], [PS[bq].b])
                    S.op("act", lambda e, j=j, bq=bq: e.activation(out=qsb[j][:], in_=PS[bq][:], func=AF.Copy, scale=SCALE),
                         r=[PS[bq].b], w=[qsb[j].b])
                    DMA("pool", qT_d[h, :, cols], qsb[j][:], [qsb[j].b], [], "dps")
                    mm_acc(PS[bk][:], [(w_k[:, c, hs], xg[k][:, c, :]) for c in range(8)], [wk_b[h // 2], xg[k].b], [PS[bk].b])
                    S.op("dve", lambda e, j=j, bk=bk: e.tensor_copy(out=ksb[j][:], in_=PS[bk][:]), r=[PS[bk].b], w=[ksb[j].b])
                    S.op("dve", lambda e, bk=bk, h=h, tg=tg: e.tensor_reduce(out=kmsum[:, h, tg * 2:(tg + 1) * 2],
                                                                             in_=PS[bk][:].rearrange("p (b t) -> p b t", b=2),
                                                                             axis=AX.X, op=ALU.add),
                         r=[PS[bk].b], w=[kmsum.b])
                    DMA("pool", kT_d[h, :, cols], ksb[j][:], [ksb[j].b], [], "dps")
                for tt in range(4):
                    i = tg * 4 + tt
                    jv = i % 2
                    for hf in range(2):
                        mm_acc(PS[4 + hf][:], [(xg[k][:, c, tt * 128:(tt + 1) * 128], w_v[:, c, hf * 512:(hf + 1) * 512]) for c in range(8)],
                               [xg[k].b, wv_b[hf]], [PS[4 + hf].b])
                    S.op("act", lambda e, jv=jv: e.copy(out=vaug[jv][:, 0:4, 0:128], in_=PS[4][:].rearrange("p (h d) -> p h d", h=4)),
                         r=[PS[4].b], w=[vaug[jv].b])
                    S.op("dve", lambda e, jv=jv: e.tensor_copy(out=vaug[jv][:, 4:8, 0:128], in_=PS[5][:].rearrange("p (h d) -> p h d", h=4)),
                         r=[PS[5].b], w=[vaug[jv].b])
                    DMA("pool", v_d[:, :, i, :].rearrange("h p d -> p h d"), vaug[jv][:], [vaug[jv].b], [], "dps")
            S.barrier()

        with ExitStack() as p3:
            relb = sb(p3, "relb", [32, 8], F32)
            b31bc = sb(p3, "b31bc", [128, 8], F32)
            negT = sb(p3, "negT", [128, 128], F32)
            pm = sb(p3, "pm", [128, 256], F32)
            psel = sb(p3, "psel", [128, 256], F32)
            esel_f = sb(p3, "esel_f", [16, 2048], F32)
            esel = sb(p3, "esel", [16, 16, 128], BF16)
            kmT = sb(p3, "kmT", [128, 8, 16], BF16)
            BT = sb(p3, "BT", [128, 8, 2, 128], F32)
            BTb = sb(p3, "BTb", [128, 8, 2, 128], BF16)
            maskT = sb(p3, "maskT", [16, S_LEN], BF16)
            btd = Buf()
            DMA("sp", relb[:], relb_d, [], [relb.b], "dc")
            DMA("sp", b31bc[:], relb_d[31, :].partition_broadcast(128), [], [b31bc.b], "dc")
            DMA("sp", negT[:], c_negT_d, [], [negT.b], "dc")
            DMA("sp", pm[:], c_pm_d, [], [pm.b], "dc")
            DMA("sp", psel[:], c_psel_d, [], [psel.b], "dc")
            DMA("sp", esel_f[:], c_esel_d, [], [esel_f.b], "dc")
            S.op("dve", lambda e: e.tensor_copy(out=esel[:].rearrange("a n k -> a (n k)"), in_=esel_f[:]), r=[esel_f.b], w=[esel.b])
            S.op("dve", lambda e: e.tensor_scalar(out=kmT[:], in0=kmsum[:], scalar1=1.0 / 256, scalar2=None, op0=ALU.mult),
                 r=[kmsum.b], w=[kmT.b])
            with ExitStack() as pb:
                oh_sb = [sb(pb, "oh_sb%d" % i, [32, 4096], F32) for i in range(2)]
                btst = [sb(pb, "btst%d" % i, [8, 4096], F32) for i in range(2)]
                btflat = bt_d.rearrange("h w k q -> h (w k q)")
                for ch in range(8):
                    kk = ch % 2
                    DMA("sp", oh_sb[kk][:], c_oh_d[:, ch * 4096:(ch + 1) * 4096], [], [oh_sb[kk].b], "dx")
                    for s8 in range(8):
                        bank = s8 % 2
                        S.op("pe", lambda e, kk=kk, s8=s8, bank=bank: e.matmul(PS[bank][0:8, :], lhsT=relb[:, :],
                                                                               rhs=oh_sb[kk][:, s8 * 512:(s8 + 1) * 512], start=True, stop=True),
                             r=[relb.b, oh_sb[kk].b], w=[PS[bank].b])
                        S.op("act", lambda e, kk=kk, s8=s8, bank=bank: e.copy(out=btst[kk][:, s8 * 512:(s8 + 1) * 512], in_=PS[bank][0:8, :]),
                             r=[PS[bank].b], w=[btst[kk].b])
                    DMA("sp", btflat[:, ch * 4096:(ch + 1) * 4096], btst[kk][:], [btst[kk].b], [btd], "dst")
                DMA("sp", BT[:], bt_d.rearrange("h w k q -> k h w q"), [btd], [BT.b], "dx")
                for h in range(8):
                    S.op("dve", lambda e, h=h: e.tensor_scalar(out=BT[:, h, :, :].rearrange("p w q -> p (w q)"),
                                                               in0=BT[:, h, :, :].rearrange("p w q -> p (w q)"),
                                                               scalar1=b31bc[:, h:h + 1], scalar2=None, op0=ALU.subtract),
                         r=[BT.b, b31bc.b], w=[BT.b])
                    S.op("dve", lambda e, h=h: e.tensor_tensor(out=BT[:, h, 0, :], in0=BT[:, h, 0, :], in1=negT[:], op=ALU.add),
                         r=[BT.b, negT.b], w=[BT.b])
                CP(BTb[:].rearrange("p h w q -> p (h w q)"), BT[:].rearrange("p h w q -> p (h w q)"), [BT.b], [BTb.b])
                S.barrier()

            qh = [sb(p3, "qh%d" % i, [128, S_LEN], BF16) for i in range(2)]
            kh = [sb(p3, "kh%d" % i, [128, S_LEN], BF16) for i in range(2)]
            vh = [sb(p3, "vh%d" % i, [128, NT, 129], BF16) for i in range(2)]
            NR = 4
            gp_sb = [sb(p3, "gp_sb%d" % i, [128, 16], F32) for i in range(NR)]
            top8 = [sb(p3, "top8%d" % i, [128, 8], F32) for i in range(NR)]
            mb = [sb(p3, "mb%d" % i, [128, 16], F32) for i in range(NR)]
            PT = [sb(p3, "PT%d" % i, [128, 512], BF16) for i in range(4)]
            tmpS = [sb(p3, "tmpS%d" % i, [128, 256], F32) for i in range(3)]
            rc = [sb(p3, "rc%d" % i, [128, 1], F32) for i in range(4)]
            attb = [sb(p3, "attb%d" % i, [128, 128], BF16) for i in range(4)]

            def load_head(h):
                kk = h % 2
                DMA("sp", qh[kk][:], qT_d[h], [], [qh[kk].b], "dq")
                DMA("sp", kh[kk][:], kT_d[h], [], [kh[kk].b], "dq")
                DMA("sp", vh[kk][:], v_d[h], [], [vh[kk].b], "dq")

            class Reg:
                def __init__(self, bank, off):
                    self.ap = PS[bank][:, off:off + 129]
                    self.b = PS[bank].b
            class Reg2(Reg):
                def __init__(self, bank, off):
                    Reg.__init__(self, bank, off)
                    self.bank = bank
            blk = [[Reg2((2 if p_ == 0 else 4) + lt // 2, (lt % 2) * 129) for p_ in range(2)] for lt in range(4)]
            m_all = [sb(p3, "m_all%d" % i, [128, NT, 16], F32) for i in range(2)]
            acc = [sb(p3, "acc%d" % i, [128, 129], F32) for i in range(8)]

            SBK = (0, 1, 6)
            load_head(0)
            stepc = 0
            for h in range(8):
                kk = h % 2
                if h + 1 < 8:
                    load_head(h + 1)
                qT_, kT_, vv_ = qh[kk], kh[kk], vh[kk]
                mh_ = m_all[kk]
                def mask_tile(hh, i):
                    qTn, mhn = qh[hh % 2], m_all[hh % 2]
                    c = i // 2
                    rr_ = i % NR
                    gcol = (i % 8) * 16
                    MM(PS[7][:, gcol:gcol + 16], qTn[:, i * 128:(i + 1) * 128], kmT[:, hh, :], [qTn.b, kmT.b], [PS[7].b])
                    TT(gp_sb[rr_][:], PS[7][:, gcol:gcol + 16], pm[:, c * 16:(c + 1) * 16], ALU.add, [PS[7].b, pm.b], [gp_sb[rr_].b])
                    S.op("dve", lambda e, rr_=rr_: e.max(out=top8[rr_][:], in_=gp_sb[rr_][:]), r=[gp_sb[rr_].b], w=[top8[rr_].b])
                    TS(mhn[:, i, :], gp_sb[rr_][:], top8[rr_][:, 3:4], None, ALU.is_ge, None, [gp_sb[rr_].b, top8[rr_].b], [mhn.b])

                if h == 0:
                    for i in range(NT):
                        mask_tile(0, i)
                steps = [(g, j) for g in range(8) for j in range(4 * g + 4)]

                def emit_S(idx, kT_=kT_, qT_=qT_, h=h):
                    g, j = steps[idx]
                    r_ = j - 4 * g
                    q0 = max(r_, 0) * 128
                    sb_ = PS[SBK[(stepc + idx) % 3]]
                    if r_ >= 0:
                        s0, w_, b0 = q0, (256 if r_ <= 2 else 128), 0
                    elif r_ == -1:
                        s0, w_, b0 = 0, 128, 128
                    else:
                        s0, w_, b0 = 0, 0, 0
                    MM(sb_[:, q0:512], kT_[:, j * 128:(j + 1) * 128], qT_[:, g * 512 + q0:(g + 1) * 512], [kT_.b, qT_.b], [sb_.b],
                       start=True, stop=True, silent=(w_ > 0))
                    if w_ > 0:
                        MM(sb_[:, s0:s0 + w_], ident[:], BTb[:, h, :, :].rearrange("p w q -> p (w q)")[:, b0:b0 + w_],
                           [ident.b, BTb.b], [sb_.b], start=False, stop=True, skip=True)

                emit_S(0)
                emit_S(1)
                pending_tr = []
                for idx, (g, j) in enumerate(steps):
                    if idx + 2 < len(steps):
                        emit_S(idx + 2)
                    if j == 0:
                        emit_casts(2 if g < 4 else 1)
                    if h + 1 < 8 and idx % 4 == 0 and idx // 4 < NT:
                        mask_tile(h + 1, idx // 4)
                    r_ = j - 4 * g
                    n = j // 2
                    q0 = max(r_, 0) * 128
                    sb_ = PS[SBK[(stepc + idx) % 3]]
                    pt = PT[(stepc + idx) % 4]
                    ts_ = tmpS[(stepc + idx) % 3]
                    ACTF(pt[:, q0:512], sb_[:, q0:512], AF.Exp, [sb_.b], [pt.b])
                    started = set()
                    post = []
                    for lt in range(max(r_, 0), 4):
                        i = 4 * g + lt
                        ac = acc[(g % 2) * 4 + lt]
                        rg = blk[lt][n % 2]
                        sp_ = (j % 2 == 1) or (j == i)
                        if j % 2 == 0:
                            st_ = rg.bank not in started
                            started.add(rg.bank)
                        else:
                            st_ = False
                        MM(rg.ap, pt[:, lt * 128:(lt + 1) * 128], vv_[:, j, :], [pt.b, vv_.b], [rg.b], start=st_, stop=sp_,
                           silent=False, skip=True)
                        post.append((lt, i, ac, rg, sp_))
                    for lt, i, ac, rg, sp_ in post:
                        if sp_:
                            if n == 0:
                                TS(ac[:], rg.ap, mh_[:, i, 0:1], None, ALU.mult, None, [rg.b, mh_.b], [ac.b])
                            else:
                                STT(ac[:], rg.ap, mh_[:, i, n:n + 1], ac[:], ALU.mult, ALU.add, [rg.b, mh_.b, ac.b], [ac.b])
                    for fn_ in pending_tr:
                        fn_()
                    pending_tr = []
                    for lt, i, ac, rg, sp_ in post:
                        if j == i:
                            RCP(rc[lt][:], ac[:, 128:129], [ac.b], [rc[lt].b])
                            TS(attb[lt][:], ac[:, 0:128], rc[lt][:], None, ALU.mult, None, [ac.b, rc[lt].b], [attb[lt].b])

                            def fin_(lt=lt, i=i, h=h):
                                pv7 = psb(7)
                                TR(pv7[:, lt * 128:(lt + 1) * 128], attb[lt][:], ident[:], [attb[lt].b, ident.b], [PS[7].b])
                                ACTC(attT[:, h, i * 128:(i + 1) * 128], pv7[:, lt * 128:(lt + 1) * 128], [PS[7].b], [attTb[i]])
                            pending_tr.append(fin_)
                for fn_ in pending_tr:
                    fn_()
                pending_tr = []
                stepc += len(steps)
                DMA("pool", att_d[h], attT[:, h, :], list(attTb), [], "dps")
            S.barrier()
        pA.close()


        xbufb = Buf()
        with ExitStack() as pz:
            zt = sb(pz, "zt", [128, 4096], BF16)
            MSET(zt[:], 0.0, [zt.b], eng="dve")
            xv = xbuf_d.rearrange("(a p) d -> p a d", p=128)
            for a in range(32):
                DMA("sp", xv[:, a * 4:(a + 1) * 4, :], zt[:].rearrange("p (a d) -> p a d", a=4), [zt.b], [xbufb], "dz")
            S.barrier()

        with ExitStack() as p4:
            w_b = sb(p4, "w_b", [128, 8, 1024], BF16)
            w_o = sb(p4, "w_o", [128, 8, 1024], BF16)
            w_xq = sb(p4, "w_xq", [128, 8, 512], BF16)
            w_xo = sb(p4, "w_xo", [128, 4, 1024], BF16)
            w_r = sb(p4, "w_r", [128, 8, 72], F32)
            b_r = sb(p4, "b_r", [128, 72], F32)
            gx = sb(p4, "gx", [128, D], F32)
            gffn = sb(p4, "gffn", [128, D], F32)
            ebase = sb(p4, "ebase", [128, 64], F32)
            ltri = sb(p4, "ltri", [128, 128], BF16)
            cum = sb(p4, "cum", [128, 64], F32)
            memKT = sb(p4, "memKT", [128, 4, 256], BF16)
            memV = sb(p4, "memV", [128, 2, 4, 129], BF16)
            DMA("pool", w_b[:], w_b_d.rearrange("(c p) n -> p c n", p=128), [], [w_b.b], "dw")
            DMA("pool", w_o[:], w_o_d.rearrange("(c p) n -> p c n", p=128), [], [w_o.b], "dw")
            DMA("pool", w_xq[:], w_xq_d.rearrange("(c p) n -> p c n", p=128), [], [w_xq.b], "dw")
            DMA("pool", w_xo[:], w_xo_d.rearrange("(c p) n -> p c n", p=128), [], [w_xo.b], "dw")
            DMA("pool", ltri[:], c_ltri_d, [], [ltri.b], "dw")
            DMA("sp", w_r[:, :, 0:8], w_rg_d.rearrange("(c p) n -> p c n", p=128), [], [w_r.b], "dc")
            DMA("sp", w_r[:, :, 8:72], w_re_d.rearrange("(c p) n -> p c n", p=128), [], [w_r.b], "dc")
            DMA("sp", b_r[:, 0:8], b_rg_d.partition_broadcast(128), [], [b_r.b], "dc")
            DMA("sp", b_r[:, 8:72], b_re_d.partition_broadcast(128), [], [b_r.b], "dc")
            DMA("sp", gx[:], norm_x_d.partition_broadcast(128), [], [gx.b], "dc")
            DMA("sp", gffn[:], norm_ffn_d.partition_broadcast(128), [], [gffn.b], "dc")
            DMA("sp", ebase[:], c_ebase_d, [], [ebase.b], "dc")
            MSET(cum[:], 0.0, [cum.b], eng="dve")
            MSET(memV[:, :, :, 128:129], 1.0, [memV.b], eng="dve")

            junk3 = sb(p4, "junk3", [128, D], F32)
            ss3 = sb(p4, "ss3", [128, 1], F32)
            rstd3 = sb(p4, "rstd3", [128, 1], F32)
            with ExitStack() as pm_:
                w_xkv = sb(pm_, "w_xkv", [128, 8, 1024], BF16)
                gmem = sb(pm_, "gmem", [128, D], F32)
                DMA("pool", w_xkv[:], w_xkv_d.rearrange("(c p) n -> p c n", p=128), [], [w_xkv.b], "dw")
                DMA("sp", gmem[:], norm_mem_d.partition_broadcast(128), [], [gmem.b], "dc")
                memt = sb(pm_, "memt", [128, D], F32)
                memn = sb(pm_, "memn", [128, D], BF16)
                memnT = sb(pm_, "memnT", [128, 8, 256], BF16)
                mT1 = sb(pm_, "mT1", [128, 8, 128], BF16)
                for mh in range(2):
                    DMA("sp", memt[:], mem_d[mh * 128:(mh + 1) * 128, :], [], [memt.b], "dx")
                    rmsnorm_rstd(memt, ss3, rstd3, junk3)
                    STT(memn[:], memt[:], rstd3[:], gmem[:], ALU.mult, ALU.mult, [memt.b, rstd3.b, gmem.b], [memn.b])
                    transpose8(memn, mT1, 7)
                    CP(memnT[:, :, mh * 128:(mh + 1) * 128], mT1[:], [mT1.b], [memnT.b])
                for h4 in range(4):
                    mm_acc(PS[h4 % 2][:, 0:256], [(w_xkv[:, c, h4 * 128:(h4 + 1) * 128], memnT[:, c, :]) for c in range(8)],
                           [w_xkv.b, memnT.b], [PS[h4 % 2].b])
                    ACTC(memKT[:, h4, :], PS[h4 % 2][:, 0:256], [PS[h4 % 2].b], [memKT.b])
                for mh in range(2):
                    mm_acc(PS[2 + mh][:], [(memnT[:, c, mh * 128:(mh + 1) * 128], w_xkv[:, c, 512:1024]) for c in range(8)],
                           [w_xkv.b, memnT.b], [PS[2 + mh].b])
                    CP(memV[:, mh, :, 0:128], PS[2 + mh][:].rearrange("p (h d) -> p h d", h=4), [PS[2 + mh].b], [memV.b])
                S.barrier()

            class WK:
                pass
            wk = []
            for p_ in range(2):
                W = WK()
                for nm, shp, dt in [("gat", [128, D], BF16), ("g1t", [128, D], BF16), ("xt3", [128, D], F32), ("att", [128, 8, 128], BF16),
                                    ("mrg_f", [128, D], F32), ("mrg_b", [128, D], BF16), ("mT", [128, 8, 128], BF16), ("h1", [128, D], F32),
                                    ("xn2", [128, D], BF16), ("xn2T", [128, 8, 128], BF16), ("q2T", [128, 4, 128], BF16),
                                    ("P2T", [128, 8, 128], BF16), ("rc2", [128, 4], F32), ("ox", [128, 512], BF16), ("oxT", [128, 4, 128], BF16),
                                    ("h2", [128, D], F32), ("xn3f", [128, D], F32), ("xn3T", [128, 8, 128], F32), ("junk", [128, D], F32),
                                    ("ss", [128, 1], F32), ("rstd", [128, 1], F32),
                                    ("lg", [128, 72], F32), ("gmax", [128, 1], F32), ("ngmax", [128, 1], F32), ("ohg", [128, 8], F32),
                                    ("eg", [128, 8], F32), ("sumg", [128, 1], F32), ("pg", [128, 1], F32), ("pen", [128, 8], F32),
                                    ("em", [128, 64], F32), ("tp8", [128, 8], F32), ("sel1", [128, 64], F32), ("sel2", [128, 64], F32),
                                    ("selb", [128, 64], BF16), ("dd", [128, 1], F32), ("ee", [128, 1], F32), ("w1", [128, 1], F32),
                                    ("w2", [128, 1], F32), ("posf", [128, 64], F32), ("over", [128, 64], F32), ("prod", [128, 64], F32),
                                    ("slf", [128, 2], F32)]:
                    setattr(W, nm, sb(p4, "%s_%d" % (nm, p_), shp, dt))
                wk.append(W)

            def tile3(i):
                p_ = i % 2
                W = wk[p_]
                A, B_, C, TB = (0, 1, 2, 3) if p_ == 0 else (4, 5, 6, 7)
                AB = (A, B_)
                rows = slice(i * 128, (i + 1) * 128)
                emit_casts(1)
                DMA("sp", W.att[:], att_d[:, :, rows].rearrange("c p t -> p c t"), [], [W.att.b], "dx")
                DMA("sp", W.gat[:], ga_d[rows, :], [], [W.gat.b], "dx")
                DMA("sp", W.g1t[:], g1_d[rows, :], [], [W.g1t.b], "dx")
                DMA("sp", W.xt3[:], x_d[rows, :], [], [W.xt3.b], "dx")
                for hf, bk in enumerate(AB):
                    mm_acc(PS[bk][:], [(W.att[:, h, :], w_b[:, h, hf * 512:(hf + 1) * 512]) for h in range(8)],
                           [W.att.b, w_b.b], [PS[bk].b])
                yield
                for hf, bk in enumerate(AB):
                    cs = slice(hf * 512, (hf + 1) * 512)
                    TT(W.mrg_f[:, cs], PS[bk][:], W.g1t[:, cs], ALU.mult, [PS[bk].b, W.g1t.b], [W.mrg_f.b])
                TT(W.mrg_b[:], W.mrg_f[:], W.gat[:], ALU.add, [W.mrg_f.b, W.gat.b], [W.mrg_b.b])
                transpose8(W.mrg_b, W.mT, TB)
                yield
                for hf, bk in enumerate(AB):
                    mm_acc(PS[bk][:], [(W.mT[:, c, :], w_o[:, c, hf * 512:(hf + 1) * 512]) for c in range(8)],
                           [W.mT.b, w_o.b], [PS[bk].b])
                yield
                for hf, bk in enumerate(AB):
                    cs = slice(hf * 512, (hf + 1) * 512)
                    TT(W.h1[:, cs], PS[bk][:], W.xt3[:, cs], ALU.add, [PS[bk].b, W.xt3.b], [W.h1.b])
                rmsnorm_rstd(W.h1, W.ss, W.rstd, W.junk)
                STT(W.xn2[:], W.h1[:], W.rstd[:], gx[:], ALU.mult, ALU.mult, [W.h1.b, W.rstd.b, gx.b], [W.xn2.b])
                transpose8(W.xn2, W.xn2T, TB, evac="dve")
                yield
                for h4 in range(4):
                    mm_acc(PS[C][:, h4 * 128:(h4 + 1) * 128], [(w_xq[:, c, h4 * 128:(h4 + 1) * 128], W.xn2T[:, c, :]) for c in range(8)],
                           [w_xq.b, W.xn2T.b], [PS[C].b])
                ACTF(W.q2T[:].rearrange("p h t -> p (h t)"), PS[C][:], AF.Copy, [PS[C].b], [W.q2T.b], scale=SCALE)
                yield
                for h4 in range(4):
                    for mh in range(2):
                        idx = h4 * 2 + mh
                        bk = AB[idx // 4]
                        MM(PS[bk][:, (idx % 4) * 128:(idx % 4 + 1) * 128], memKT[:, h4, mh * 128:(mh + 1) * 128], W.q2T[:, h4, :],
                           [memKT.b, W.q2T.b], [PS[bk].b], silent=(idx % 4 != 3))
                for bb in range(2):
                    ACTF(W.P2T[:, bb * 4:(bb + 1) * 4, :].rearrange("p a t -> p (a t)"), PS[AB[bb]][:], AF.Exp, [PS[AB[bb]].b], [W.P2T.b])
                yield
                for h4 in range(4):
                    bk = AB[h4 // 2]
                    off = (h4 % 2) * 129
                    for mh in range(2):
                        MM(PS[bk][:, off:off + 129], W.P2T[:, h4 * 2 + mh, :], memV[:, mh, h4, :], [W.P2T.b, memV.b], [PS[bk].b],
                           start=(mh == 0), stop=(mh == 1), silent=(mh == 0))
                for h4 in range(4):
                    bk = AB[h4 // 2]
                    off = (h4 % 2) * 129
                    RCP(W.rc2[:, h4:h4 + 1], PS[bk][:, off + 128:off + 129], [PS[bk].b], [W.rc2.b])
                    TS(W.ox[:, h4 * 128:(h4 + 1) * 128], PS[bk][:, off:off + 128], W.rc2[:, h4:h4 + 1], None, ALU.mult, None,
                       [PS[bk].b, W.rc2.b], [W.ox.b])
                transpose8(W.ox, W.oxT, TB, n=4)
                yield
                for hf, bk in enumerate(AB):
                    mm_acc(PS[bk][:], [(W.oxT[:, c, :], w_xo[:, c, hf * 512:(hf + 1) * 512]) for c in range(4)],
                           [W.oxT.b, w_xo.b], [PS[bk].b])
                for hf, bk in enumerate(AB):
                    cs = slice(hf * 512, (hf + 1) * 512)
                    TT(W.h2[:, cs], PS[bk][:], W.h1[:, cs], ALU.add, [PS[bk].b, W.h1.b], [W.h2.b])
                DMA("pool", h2_d[rows, :], W.h2[:], [W.h2.b], [], "dps")
                yield
                rmsnorm_rstd(W.h2, W.ss, W.rstd, W.junk)
                STT(W.xn3f[:], W.h2[:], W.rstd[:], gffn[:], ALU.mult, ALU.mult, [W.h2.b, W.rstd.b, gffn.b], [W.xn3f.b])
                ACTC(xn3b[p_][:], W.xn3f[:], [W.xn3f.b], [xn3b[p_].b])
                for c in range(8):
                    bk = AB[c // 4]
                    TR(PS[bk][:, (c % 4) * 128:(c % 4 + 1) * 128], W.xn3f[:, c * 128:(c + 1) * 128], identf[:], [W.xn3f.b, identf.b], [PS[bk].b],
                       silent=(c % 4 != 3))
                CP(W.xn3T[:, 0:4, :].rearrange("p c t -> p (c t)"), PS[A][:], [PS[A].b], [W.xn3T.b])
                ACTC(W.xn3T[:, 4:8, :].rearrange("p c t -> p (c t)"), PS[B_][:], [PS[B_].b], [W.xn3T.b])
                yield
                mm_acc(PS[C][:, 0:72], [(W.xn3T[:, c, :], w_r[:, c, :]) for c in range(8)], [W.xn3T.b, w_r.b], [PS[C].b])
                TT(W.lg[:], PS[C][:, 0:72], b_r[:], ALU.add, [PS[C].b, b_r.b], [W.lg.b])
                RED(W.gmax[:], W.lg[:, 0:8], ALU.max, [W.lg.b], [W.gmax.b])
                TS(W.ohg[:], W.lg[:, 0:8], W.gmax[:], None, ALU.is_equal, None, [W.lg.b, W.gmax.b], [W.ohg.b])
                TS(W.ngmax[:], W.gmax[:], -1.0, None, ALU.mult, None, [W.gmax.b], [W.ngmax.b])
                ACTF(W.eg[:], W.lg[:, 0:8], AF.Exp, [W.lg.b, W.ngmax.b], [W.eg.b, W.sumg.b], bias=W.ngmax[:], accum_out=W.sumg[:])
                RCP(W.pg[:], W.sumg[:], [W.sumg.b], [W.pg.b])
                TS(W.pen[:], W.ohg[:], 1e30, -1e30, ALU.mult, ALU.add, [W.ohg.b], [W.pen.b])
                TT(W.em[:].rearrange("p (g e) -> p g e", g=8), W.lg[:, 8:72].rearrange("p (g e) -> p g e", g=8),
                   W.pen[:, :].unsqueeze(2).to_broadcast([128, 8, 8]), ALU.add, [W.lg.b, W.pen.b], [W.em.b])
                S.op("dve", lambda e, W=W: e.max(out=W.tp8[:], in_=W.em[:]), r=[W.em.b], w=[W.tp8.b])
                TS(W.sel1[:], W.em[:], W.tp8[:, 0:1], None, ALU.is_equal, None, [W.em.b, W.tp8.b], [W.sel1.b])
                TS(W.sel2[:], W.em[:], W.tp8[:, 1:2], None, ALU.is_equal, None, [W.em.b, W.tp8.b], [W.sel2.b])
                TT(W.dd[:], W.tp8[:, 1:2], W.tp8[:, 0:1], ALU.subtract, [W.tp8.b], [W.dd.b])
                ACTF(W.ee[:], W.dd[:], AF.Exp, [W.dd.b], [W.ee.b])
                TS(W.w1[:], W.ee[:], 1.0, None, ALU.add, None, [W.ee.b], [W.w1.b])
                RCP(W.w1[:], W.w1[:], [W.w1.b], [W.w1.b])
                TS(W.w2[:], W.w1[:], -1.0, 1.0, ALU.mult, ALU.add, [W.w1.b], [W.w2.b])
                TT(wts[:, i, 0:1], W.w1[:], W.pg[:], ALU.mult, [W.w1.b, W.pg.b], [wts.b])
                TT(wts[:, i, 1:2], W.w2[:], W.pg[:], ALU.mult, [W.w2.b, W.pg.b], [wts.b])
                TT(W.selb[:], W.sel1[:], W.sel2[:], ALU.add, [W.sel1.b, W.sel2.b], [W.selb.b])
                MM(PS[C][:, 128:192], ltri[:], W.selb[:], [ltri.b, W.selb.b], [PS[C].b])
                MM(PS[C][:, 256:320], onesb[:], W.selb[:], [onesb.b, W.selb.b], [PS[C].b])
                TT(W.posf[:], PS[C][:, 128:192], cum[:], ALU.add, [PS[C].b, cum.b], [W.posf.b])
                TT(cum[:], cum[:], PS[C][:, 256:320], ALU.add, [PS[C].b, cum.b], [cum.b])
                TS(W.over[:], W.posf[:], float(CAP), 1e6, ALU.is_ge, ALU.mult, [W.posf.b], [W.over.b])
                TT(W.posf[:], W.posf[:], W.over[:], ALU.add, [W.posf.b, W.over.b], [W.posf.b])
                TT(W.posf[:], W.posf[:], ebase[:], ALU.add, [W.posf.b, ebase.b], [W.posf.b])
                TT(W.prod[:], W.posf[:], W.sel1[:], ALU.mult, [W.posf.b, W.sel1.b], [W.prod.b])
                RED(W.slf[:, 0:1], W.prod[:], ALU.add, [W.prod.b], [W.slf.b])
                TT(W.prod[:], W.posf[:], W.sel2[:], ALU.mult, [W.posf.b, W.sel2.b], [W.prod.b])
                RED(W.slf[:, 1:2], W.prod[:], ALU.add, [W.prod.b], [W.slf.b])
                CP(slots[:, 2 * i:2 * i + 2], W.slf[:], [W.slf.b], [slots.b])
                for kk in range(2):
                    S.op("pool", lambda e, i=i, kk=kk, p_=p_: e.indirect_dma_start(
                        out=xbuf_d, out_offset=bass.IndirectOffsetOnAxis(ap=slots[:, 2 * i + kk:2 * i + kk + 1], axis=0),
                        in_=xn3b[p_][:, :], in_offset=None, bounds_check=S.regs["bnd"], oob_is_err=False),
                        r=[xn3b[p_].b, slots.b], w=[xbufb], sem="dg", inc=16)
                yield

            SKEW = 5
            active = []
            nxt = 0
            while nxt < NT or active:
                if nxt < NT and len(active) < 2 and (not active or active[-1][1] >= SKEW):
                    active.append([tile3(nxt), 0])
                    nxt += 1
                for a_ in list(active):
                    try:
                        next(a_[0])
                        a_[1] += 1
                    except StopIteration:
                        active.remove(a_)
            emit_casts(10 ** 6)
            if debug:
                DMA("sp", dbg_rt[:, :, 0:2], wts[:], [wts.b], [], "dst")
            S.barrier()

        with ExitStack() as p5:
            NWB = 3
            wg = [sb(p5, "wg%d" % i, [128, 8, 512], BF16) for i in range(NWB)]
            wu = [sb(p5, "wu%d" % i, [128, 8, 512], BF16) for i in range(NWB)]
            wd = [sb(p5, "wd%d" % i, [128, 4, 1024], BF16) for i in range(NWB)]
            xe = [sb(p5, "xe%d" % i, [128, D], BF16) for i in range(4)]
            xeT = [sb(p5, "xeT%d" % i, [128, 8, 256], BF16) for i in range(2)]
            sgl = sb(p5, "sgl", [128, 1024], F32)
            hdn = [sb(p5, "hdn%d" % i, [128, 4, 256], BF16) for i in range(2)]
            yb = [sb(p5, "yb%d" % i, [128, D], BF16) for i in range(2)]
            def load_x(ex):
                for blk in range(2):
                    xb = xe[(2 * ex + blk) % 4]
                    r0 = ex * CAP + blk * 128
                    DMA("sp", xb[:], xbuf_d[r0:r0 + 128, :], [], [xb.b], "dx")

            def load_w(ex_):
                kw = ex_ % NWB
                DMA("pool", wg[kw][:].rearrange("p c n -> p (c n)"), wgb_d[ex_], [], [wg[kw].b], "dw")
                DMA("act", wu[kw][:].rearrange("p c n -> p (c n)"), wub_d[ex_], [], [wu[kw].b], "dq")
                DMA("sp", wd[kw][:].rearrange("p c n -> p (c n)"), wdb_d[ex_], [], [wd[kw].b], "dc")

            load_w(0)
            load_w(1)
            for ex in range(NEXP):
                k = ex % NWB
                k2 = ex % 2
                if ex + 2 < NEXP:
                    load_w(ex + 2)
                if ex == 0:
                    load_x(0)
                if ex + 1 < NEXP:
                    load_x(ex + 1)
                for blk in range(2):
                    xb = xe[(2 * ex + blk) % 4]
                    pv = psb(7)
                    for c in range(8):
                        TR(pv[:, c * 128:(c + 1) * 128], xb[:, c * 128:(c + 1) * 128], ident[:], [xb.b, ident.b], [PS[7].b], silent=(c < 7))
                    if blk == 0:
                        ACTC(xeT[k2][:, :, 0:128], pv[:, :].rearrange("p (c t) -> p c t", c=8), [PS[7].b], [xeT[k2].b])
                    else:
                        CP(xeT[k2][:, :, 128:256], pv[:, :].rearrange("p (c t) -> p c t", c=8), [PS[7].b], [xeT[k2].b])
                for hc in range(4):
                    cs = slice((hc % 2) * 256, (hc % 2 + 1) * 256)
                    mm_acc(PS[hc // 2][:, cs], [(wg[k][:, c, hc * 128:(hc + 1) * 128], xeT[k2][:, c, :]) for c in range(8)],
                           [wg[k].b, xeT[k2].b], [PS[hc // 2].b])
                for hc in range(4):
                    cs = slice((hc % 2) * 256, (hc % 2 + 1) * 256)
                    mm_acc(PS[2 + hc // 2][:, cs], [(wu[k][:, c, hc * 128:(hc + 1) * 128], xeT[k2][:, c, :]) for c in range(8)],
                           [wu[k].b, xeT[k2].b], [PS[2 + hc // 2].b])
                for b2 in range(2):
                    cs = slice(b2 * 512, (b2 + 1) * 512)
                    ACTF(sgl[:, cs], PS[b2][:], AF.Silu, [PS[b2].b], [sgl.b])
                    TT(hdn[k2][:, 2 * b2:2 * b2 + 2, :].rearrange("p a t -> p (a t)"), sgl[:, cs], PS[2 + b2][:], ALU.mult,
                       [sgl.b, PS[2 + b2].b], [hdn[k2].b])
                for blk in range(2):
                    ybk = yb[blk]
                    for hf in range(2):
                        mm_acc(PS[4 + hf][:], [(hdn[k2][:, hc, blk * 128:(blk + 1) * 128], wd[k][:, hc, hf * 512:(hf + 1) * 512]) for hc in range(4)],
                               [hdn[k2].b, wd[k].b], [PS[4 + hf].b])
                    ACTC(ybk[:, 0:512], PS[4][:], [PS[4].b], [ybk.b])
                    CP(ybk[:, 512:1024], PS[5][:], [PS[5].b], [ybk.b])
                    r0 = ex * CAP + blk * 128
                    DMA("sp", ybuf_d[r0:r0 + 128, :], ybk[:], [ybk.b], [], "dst")
            S.barrier()

        with ExitStack() as p6:
            h2t = [sb(p6, "h2t%d" % i, [128, D], F32) for i in range(2)]
            h3 = sb(p6, "h3", [128, D], F32)
            junk5 = sb(p6, "junk5", [128, D], F32)
            ss5 = sb(p6, "ss5", [128, 1], F32)
            rstd5 = sb(p6, "rstd5", [128, 1], F32)
            ot = [sb(p6, "ot%d" % i, [128, D], F32) for i in range(2)]
            for i in range(NT):
                k = i % 2
                rows = slice(i * 128, (i + 1) * 128)
                DMA("sp", h2t[k][:], h2_d[rows, :], [], [h2t[k].b], "dx")
                MSET(y1[k][:], 0.0, [y1[k].b])
                MSET(y2[k][:], 0.0, [y2[k].b])
                for kk, yy in ((0, y1[k]), (1, y2[k])):
                    S.op("pool", lambda e, i=i, kk=kk, yy=yy: e.indirect_dma_start(
                        out=yy[:, :], out_offset=None, in_=ybuf_d,
                        in_offset=bass.IndirectOffsetOnAxis(ap=slots[:, 2 * i + kk:2 * i + kk + 1], axis=0),
                        bounds_check=S.regs["bnd"], oob_is_err=False),
                        r=[slots.b], w=[yy.b], sem="dg", inc=16)
                STT(h3[:], y1[k][:], wts[:, i, 0:1], h2t[k][:], ALU.mult, ALU.add, [y1[k].b, wts.b, h2t[k].b], [h3.b])
                STT(h3[:], y2[k][:], wts[:, i, 1:2], h3[:], ALU.mult, ALU.add, [y2[k].b, wts.b, h3.b], [h3.b])
                rmsnorm_rstd(h3, ss5, rstd5, junk5)
                STT(ot[k][:], h3[:], rstd5[:], gfin[:], ALU.mult, ALU.mult, [h3.b, rstd5.b, gfin.b], [ot[k].b])
                DMA("act", out_d[rows, :], ot[k][:], [ot[k].b], [], "dst")

        block = es.enter_context(nc.Block())
        S.flush(block)
    return nc


_NC_CACHE = {}


def kernel(**inputs):
    f32 = lambda a: np.ascontiguousarray(np.asarray(a), dtype=np.float32)
    x = f32(inputs["x"])
    mem = f32(inputs["mem"])
    B = x.shape[0]
    shared = {}
    for k, v in inputs.items():
        if k in ("x", "mem"):
            continue
        a = f32(v)
        if k in ("rel_bias", "norm_final"):
            shared[k] = a
        else:
            shared[k] = np.ascontiguousarray(a[0])
    shared.update({k: f32(v) for k, v in host_consts().items()})
    if "nc" not in _NC_CACHE:
        _NC_CACHE["nc"] = build(debug=False)
    nc = _NC_CACHE["nc"]
    in_maps = []
    for b in range(B):
        m = dict(shared)
        m["x"] = np.ascontiguousarray(x[b])
        m["mem"] = np.ascontiguousarray(mem[b])
        in_maps.append(m)
    res = run_bass_kernel_spmd(nc, in_maps, core_ids=list(range(B)))
    out = np.stack([np.asarray(r["out"], dtype=np.float32) for r in res.results], axis=0)
    return out
```
